# Optimizing a Trainium2 kernel written in Bass

```python
import math
import jax, jax.numpy as jnp
from jax import lax
import numpy as np

D_MODEL = 4096
BATCH = 1
SEQ = 8192
DEPTH = 2

N_MIXERS = 2
N_POOL_LAYERS = (DEPTH + 1) // 2
N_SSD_LAYERS = DEPTH // 2

DEEPNORM_ALPHA = (2 * DEPTH) ** 0.25
DEEPNORM_BETA = (8 * DEPTH) ** -0.25
LN_EPS = 1e-5

POOL_WINDOWS = (2, 4, 8, 16)
POOL_GROUPS = len(POOL_WINDOWS)
POOL_GC = D_MODEL // POOL_GROUPS

SSD_EXPAND = 2
D_INNER = SSD_EXPAND * D_MODEL
SSD_HEAD_DIM = 64
SSD_HEADS = D_INNER // SSD_HEAD_DIM
SSD_GROUPS = 8
SSD_HPG = SSD_HEADS // SSD_GROUPS
SSD_STATE = 128
SSD_CONV = 4
SSD_CHUNK = 128
SSD_CONV_DIM = D_INNER + 2 * SSD_GROUPS * SSD_STATE
SSD_IN_DIM = D_INNER + SSD_CONV_DIM + SSD_HEADS
RMS_EPS = 1e-5

N_EXPERTS = 32
N_EXPERT_GROUPS = 8
EXPERTS_PER_GROUP = N_EXPERTS // N_EXPERT_GROUPS
TOP_K = 2
D_FF = 768
BLOCK_ROWS = 128

kernel_name = 'hybrid_pool_ssd_moe'


def layer_norm(x, g, b):
    xf = x.astype(jnp.float32)
    mu = jnp.mean(xf, axis=-1, keepdims=True)
    xc = xf - mu
    var = jnp.mean(xc * xc, axis=-1, keepdims=True)
    y = xc * lax.rsqrt(var + LN_EPS) * g.astype(jnp.float32) + b.astype(jnp.float32)
    return y.astype(x.dtype)


def pool_mixer(x, w_in, w_group, scale, w_out):
    b, l, d = x.shape
    u = (x @ w_in).astype(jnp.float32)
    csum = jnp.cumsum(u, axis=1)
    pos = jnp.arange(1, l + 1, dtype=jnp.float32)
    parts = []
    for g, w in enumerate(POOL_WINDOWS):
        c = csum[..., g * POOL_GC:(g + 1) * POOL_GC]
        lag = jnp.pad(c, ((0, 0), (w, 0), (0, 0)))[:, :l]
        mean = (c - lag) / jnp.minimum(pos, float(w))[None, :, None]
        parts.append(mean - u[..., g * POOL_GC:(g + 1) * POOL_GC])
    p = jnp.stack(parts, axis=2).astype(x.dtype)
    mixed = jnp.einsum('blgc,gcd->blgd', p, w_group).reshape(b, l, d) * scale
    return mixed @ w_out


def causal_depthwise_conv(u, w, bias):
    c = u.shape[-1]
    out = lax.conv_general_dilated(
        u, w[:, None, :], window_strides=(1,), padding=[(SSD_CONV - 1, 0)],
        dimension_numbers=('NWC', 'WIO', 'NWC'), feature_group_count=c)
    return out + bias


def ssd_chunked(xs, dt, a, bmat, cmat):
    b, l = xs.shape[:2]
    nc = l // SSD_CHUNK
    q = SSD_CHUNK
    xs = xs.reshape(b, nc, q, SSD_GROUPS, SSD_HPG, SSD_HEAD_DIM)
    dt = dt.reshape(b, nc, q, SSD_GROUPS, SSD_HPG)
    bmat = bmat.reshape(b, nc, q, SSD_GROUPS, SSD_STATE)
    cmat = cmat.reshape(b, nc, q, SSD_GROUPS, SSD_STATE)
    a_cum = jnp.cumsum(dt * a, axis=2)
    a_t = jnp.moveaxis(a_cum, 2, -1)
    dt_t = jnp.moveaxis(dt, 2, -1)
    mask = jnp.tril(jnp.ones((q, q), dtype=bool))
    seg = a_t[..., :, None] - a_t[..., None, :]
    cb = jnp.einsum('bclgn,bcsgn->bcgls', cmat, bmat)
    m = cb[:, :, :, None] * jnp.exp(jnp.where(mask, seg, -jnp.inf)) * dt_t[..., None, :]
    y_diag = jnp.einsum('bcghls,bcsghp->bclghp', m, xs)
    decay_states = jnp.exp(a_cum[:, :, -1:] - a_cum)
    states = jnp.einsum('bcsgn,bcsgh,bcsghp->bcghpn', bmat, decay_states * dt, xs)
    chunk_decay = jnp.exp(a_cum[:, :, -1])

    def step(h, inp):
        s_c, d_c = inp
        return d_c[..., None, None] * h + s_c, h

    h0 = jnp.zeros((b, SSD_GROUPS, SSD_HPG, SSD_HEAD_DIM, SSD_STATE), jnp.float32)
    _, prev = lax.scan(step, h0, (jnp.moveaxis(states, 1, 0), jnp.moveaxis(chunk_decay, 1, 0)))
    prev = jnp.moveaxis(prev, 0, 1)
    y_off = jnp.einsum('bclgn,bcghpn,bclgh->bclghp', cmat, prev, jnp.exp(a_cum))
    return (y_diag + y_off).reshape(b, l, SSD_GROUPS, SSD_HPG, SSD_HEAD_DIM)


def ssd_mixer(x, w_in, conv_w, conv_b, dt_bias, a_log, d_skip, norm_w, w_out):
    b, l, _ = x.shape
    zxbcdt = x @ w_in
    z = zxbcdt[..., :D_INNER]
    xbc = zxbcdt[..., D_INNER:D_INNER + SSD_CONV_DIM]
    dt = zxbcdt[..., D_INNER + SSD_CONV_DIM:]
    xbc = jax.nn.silu(causal_depthwise_conv(xbc, conv_w, conv_b)).astype(jnp.float32)
    gn = SSD_GROUPS * SSD_STATE
    xs = xbc[..., :D_INNER].reshape(b, l, SSD_GROUPS, SSD_HPG, SSD_HEAD_DIM)
    bmat = xbc[..., D_INNER:D_INNER + gn].reshape(b, l, SSD_GROUPS, SSD_STATE)
    cmat = xbc[..., D_INNER + gn:].reshape(b, l, SSD_GROUPS, SSD_STATE)
    dt = jax.nn.softplus(dt.astype(jnp.float32) + dt_bias.astype(jnp.float32))
    dt = dt.reshape(b, l, SSD_GROUPS, SSD_HPG)
    a = -jnp.exp(a_log.astype(jnp.float32)).reshape(SSD_GROUPS, SSD_HPG)
    y = ssd_chunked(xs, dt, a, bmat, cmat)
    y = y + d_skip.astype(jnp.float32).reshape(SSD_GROUPS, SSD_HPG)[:, :, None] * xs
    y = y.reshape(b, l, D_INNER) * jax.nn.silu(z.astype(jnp.float32))
    yg = y.reshape(b, l, SSD_GROUPS, D_INNER // SSD_GROUPS)
    yg = yg * lax.rsqrt(jnp.mean(yg * yg, axis=-1, keepdims=True) + RMS_EPS)
    y = yg.reshape(b, l, D_INNER) * norm_w.astype(jnp.float32)
    return y.astype(x.dtype) @ w_out


def moe_ffn(x, w_router, router_bias, w_in, w_out):
    b, l, d = x.shape
    t = b * l
    tk = t * TOP_K
    xf = x.reshape(t, d)
    scores = jax.nn.sigmoid((xf @ w_router).astype(jnp.float32))
    sel = scores + router_bias.astype(jnp.float32)
    grp = sel.reshape(t, N_EXPERT_GROUPS, EXPERTS_PER_GROUP)
    group_score = jnp.sum(lax.top_k(grp, 2)[0], axis=-1)
    g_idx = jnp.argmax(group_score, axis=-1).astype(jnp.int32)
    in_grp = jnp.take_along_axis(grp, g_idx[:, None, None], axis=1)[:, 0]
    _, local = lax.top_k(in_grp, TOP_K)
    expert_idx = g_idx[:, None] * EXPERTS_PER_GROUP + local.astype(jnp.int32)
    gate = jnp.take_along_axis(scores, expert_idx, axis=1)
    gate = gate / jnp.sum(gate, axis=-1, keepdims=True)
    flat_e = expert_idx.reshape(tk)
    flat_tok = jnp.arange(tk, dtype=jnp.int32) // TOP_K
    counts = jnp.zeros((N_EXPERTS,), jnp.int32).at[flat_e].add(1)
    padded = (counts + BLOCK_ROWS - 1) // BLOCK_ROWS * BLOCK_ROWS
    pad_end = jnp.cumsum(padded)
    pad_start = pad_end - padded
    raw_start = jnp.cumsum(counts) - counts
    order = jnp.argsort(flat_e)
    sorted_e = flat_e[order]
    dest_sorted = pad_start[sorted_e] + jnp.arange(tk, dtype=jnp.int32) - raw_start[sorted_e]
    dest = jnp.zeros((tk,), jnp.int32).at[order].set(dest_sorted)
    n_blocks = (tk + N_EXPERTS * (BLOCK_ROWS - 1) + BLOCK_ROWS - 1) // BLOCK_ROWS
    rows = n_blocks * BLOCK_ROWS
    x_disp = jnp.zeros((rows, d), x.dtype).at[dest].set(xf[flat_tok])
    block_start = jnp.arange(n_blocks, dtype=jnp.int32) * BLOCK_ROWS
    block_expert = jnp.minimum(
        jnp.sum(block_start[:, None] >= pad_end[None, :], axis=1), N_EXPERTS - 1).astype(jnp.int32)

    def expert_block(args):
        xb, e = args
        h = xb @ w_in[e]
        return (jax.nn.silu(h[:, :D_FF]) * h[:, D_FF:]) @ w_out[e]

    y_blocks = lax.map(expert_block, (x_disp.reshape(n_blocks, BLOCK_ROWS, d), block_expert))
    y_assign = y_blocks.reshape(rows, d)[dest].reshape(t, TOP_K, d)
    y = jnp.einsum('tkd,tk->td', y_assign, gate.astype(x.dtype))
    return y.reshape(b, l, d)


def setup_inputs(seed: int = 0) -> dict:
    key = jax.random.key(seed)
    ks = jax.random.split(key, 24)
    f32 = jnp.float32
    nrm = lambda k, shape, s: jax.random.normal(k, shape, f32) * s
    x = jax.random.normal(ks[0], (BATCH, SEQ, D_MODEL), f32)
    pool_w_in = nrm(ks[1], (N_POOL_LAYERS, D_MODEL, D_MODEL), D_MODEL ** -0.5)
    pool_w_group = nrm(ks[2], (N_POOL_LAYERS, POOL_GROUPS, POOL_GC, POOL_GC), POOL_GC ** -0.5)
    pool_scale = 1.0 + nrm(ks[3], (N_POOL_LAYERS, D_MODEL), 0.1)
    pool_w_out = nrm(ks[4], (N_POOL_LAYERS, D_MODEL, D_MODEL), DEEPNORM_BETA * D_MODEL ** -0.5)
    ssd_w_in = nrm(ks[5], (N_SSD_LAYERS, D_MODEL, SSD_IN_DIM), D_MODEL ** -0.5)
    ssd_conv_w = nrm(ks[6], (N_SSD_LAYERS, SSD_CONV, SSD_CONV_DIM), SSD_CONV ** -0.5)
    ssd_conv_b = nrm(ks[7], (N_SSD_LAYERS, SSD_CONV_DIM), 0.02)
    dt0 = jnp.exp(jax.random.uniform(ks[8], (N_SSD_LAYERS, SSD_HEADS), f32,
                                     minval=math.log(1e-3), maxval=math.log(1e-1)))
    ssd_dt_bias = dt0 + jnp.log(-jnp.expm1(-dt0))
    ssd_a_log = jnp.log(jax.random.uniform(ks[9], (N_SSD_LAYERS, SSD_HEADS), f32, minval=1.0, maxval=16.0))
    ssd_d = 1.0 + nrm(ks[10], (N_SSD_LAYERS, SSD_HEADS), 0.1)
    ssd_norm_w = 1.0 + nrm(ks[11], (N_SSD_LAYERS, D_INNER), 0.1)
    ssd_w_out = nrm(ks[12], (N_SSD_LAYERS, D_INNER, D_MODEL), DEEPNORM_BETA * D_INNER ** -0.5)
    moe_w_router = nrm(ks[13], (D_MODEL, N_EXPERTS), D_MODEL ** -0.5)
    moe_router_bias = nrm(ks[14], (N_EXPERTS,), 0.01)
    moe_w_in = nrm(ks[15], (DEPTH, N_EXPERTS, D_MODEL, 2 * D_FF), D_MODEL ** -0.5)
    moe_w_out = nrm(ks[16], (DEPTH, N_EXPERTS, D_FF, D_MODEL), DEEPNORM_BETA * D_FF ** -0.5)
    ln_mix_g = 1.0 + nrm(ks[17], (DEPTH, D_MODEL), 0.1)
    ln_mix_b = nrm(ks[18], (DEPTH, D_MODEL), 0.02)
    ln_ffn_g = 1.0 + nrm(ks[19], (DEPTH, D_MODEL), 0.1)
    ln_ffn_b = nrm(ks[20], (DEPTH, D_MODEL), 0.02)
    return {'x': x, 'pool_w_in': pool_w_in, 'pool_w_group': pool_w_group, 'pool_scale': pool_scale,
            'pool_w_out': pool_w_out, 'ssd_w_in': ssd_w_in, 'ssd_conv_w': ssd_conv_w,
            'ssd_conv_b': ssd_conv_b, 'ssd_dt_bias': ssd_dt_bias, 'ssd_a_log': ssd_a_log,
            'ssd_d': ssd_d, 'ssd_norm_w': ssd_norm_w, 'ssd_w_out': ssd_w_out,
            'moe_w_router': moe_w_router, 'moe_router_bias': moe_router_bias,
            'moe_w_in': moe_w_in, 'moe_w_out': moe_w_out, 'ln_mix_g': ln_mix_g,
            'ln_mix_b': ln_mix_b, 'ln_ffn_g': ln_ffn_g, 'ln_ffn_b': ln_ffn_b}


def reference(x, pool_w_in, pool_w_group, pool_scale, pool_w_out, ssd_w_in, ssd_conv_w,
              ssd_conv_b, ssd_dt_bias, ssd_a_log, ssd_d, ssd_norm_w, ssd_w_out,
              moe_w_router, moe_router_bias, moe_w_in, moe_w_out,
              ln_mix_g, ln_mix_b, ln_ffn_g, ln_ffn_b):
    for i in range(DEPTH):
        j = i // N_MIXERS
        if i % N_MIXERS == 0:
            h = pool_mixer(x, pool_w_in[j], pool_w_group[j], pool_scale[j], pool_w_out[j])
        else:
            h = ssd_mixer(x, ssd_w_in[j], ssd_conv_w[j], ssd_conv_b[j], ssd_dt_bias[j],
                          ssd_a_log[j], ssd_d[j], ssd_norm_w[j], ssd_w_out[j])
        x = layer_norm(DEEPNORM_ALPHA * x + h, ln_mix_g[i], ln_mix_b[i])
        h = moe_ffn(x, moe_w_router, moe_router_bias, moe_w_in[i], moe_w_out[i])
        x = layer_norm(DEEPNORM_ALPHA * x + h, ln_ffn_g[i], ln_ffn_b[i])
    return x
```

```python
import contextlib
import numpy as np
import concourse.bass as bass
import concourse.mybir as mybir
from concourse.bass_utils import run_bass_kernel_spmd

F32 = mybir.dt.float32
BF16 = mybir.dt.bfloat16
I32 = mybir.dt.int32
AF = mybir.ActivationFunctionType
ALU = mybir.AluOpType
AX = mybir.AxisListType

SAME_ENGINE_SYNC = True


class Tile:
    def __init__(self, prog, handle, name):
        self.prog = prog
        self.h = handle
        self.name = name
        self.last_w = None
        self.readers = []
        self.sem = None
        self.cnt = 0
        self.excl = False
        self.multi = False
        self.writers = []

    def __getitem__(self, k):
        return self.h[k]

    def ap(self):
        return self.h[:] if not hasattr(self.h, "ap") or not callable(getattr(self.h, "ap")) else self.h.ap()


class Prog:
    ENG = ("pe", "act", "dve", "pool", "sp")

    def __init__(self, nc):
        self.nc = nc
        self.stack = contextlib.ExitStack()
        self.ops = {e: [] for e in self.ENG}
        self.count = {e: 0 for e in self.ENG}
        self.known = {e: {} for e in self.ENG}
        self.esem = {}
        for e in self.ENG:
            self.esem[e] = self.stack.enter_context(nc.semaphore("c_" + e))
        self.free_sems = []
        self._dma_tiles = []
        self._cc_tiles = []
        self.nsem = 0
        self.uid = 0

    def sb(self, shape, dtype, name=None, stack=None):
        self.uid += 1
        name = (name or "t") + "_%d" % self.uid
        h = (stack or self.stack).enter_context(self.nc.sbuf_tensor(name, list(shape), dtype))
        return Tile(self, h, name)

    def ps(self, shape, dtype, name=None, stack=None):
        self.uid += 1
        name = (name or "p") + "_%d" % self.uid
        h = (stack or self.stack).enter_context(self.nc.psum_tensor(name, list(shape), dtype))
        t = Tile(self, h, name)
        t.excl = True
        return t

    def dram(self, name, shape, dtype, kind="Internal"):
        h = self.nc.dram_tensor(name, list(shape), dtype, kind=kind)
        t = Tile(self, h, name)
        t.multi = True
        return t

    def _sem_for(self, tile):
        if tile.sem is None:
            if self.free_sems:
                tile.sem, tile.cnt = self.free_sems.pop()
            else:
                self.nsem += 1
                tile.sem = self.stack.enter_context(self.nc.semaphore("d%d" % self.nsem))
                tile.cnt = 0
            self._dma_tiles.append(tile)
        return tile.sem

    def _deps(self, eng, reads, writes, is_dma):
        deps = []
        reads, writes = self._rw(reads, writes)
        for t in reads:
            if t.multi:
                deps.extend(t.writers)
            elif t.last_w is not None:
                deps.append(t.last_w)
        for t in writes:
            if t.last_w is not None and not t.multi:
                deps.append(t.last_w)
            for r in t.readers:
                if r[0] == "eng" and r[1] == eng and not is_dma:
                    continue
                deps.append(r)
        waits = {}
        for d in deps:
            if d[0] == "eng":
                _, e2, k = d
                if e2 == eng and not is_dma and not SAME_ENGINE_SYNC:
                    continue
                key = ("eng", e2)
            else:
                _, sem, k = d
                key = ("dma", sem)
            if self.known[eng].get(key, 0) >= k:
                continue
            if waits.get(key, 0) < k:
                waits[key] = k
        for key, k in waits.items():
            self.known[eng][key] = k
        out = []
        for key, k in waits.items():
            if key[0] == "eng":
                out.append((self.esem[key[1]], k))
            else:
                out.append((key[1], k))
        return out

    @staticmethod
    def _rw(reads, writes):
        r = [t for t in reads if not t.excl]
        w = list(writes) + [t for t in reads if t.excl]
        return r, w

    def _mark(self, token, reads, writes):
        reads, writes = self._rw(reads, writes)
        for t in reads:
            t.readers.append(token)
        for t in writes:
            t.last_w = token
            t.readers = []
            if t.multi:
                t.writers.append(token)

    def op(self, eng, fns, reads=(), writes=()):
        if callable(fns):
            fns = [fns]
        waits = self._deps(eng, reads, writes, False)
        self.count[eng] += 1
        k = self.count[eng]
        self.ops[eng].append(("op", waits, fns, k))
        self._mark(("eng", eng, k), reads, writes)

    def dma(self, eng, fn, owner, reads=(), writes=(), inc=16):
        waits = self._deps(eng, reads, writes, True)
        if inc != 16:
            if owner.sem is None:
                self.nsem += 1
                owner.sem = self.stack.enter_context(self.nc.semaphore("cc%d" % self.nsem))
                owner.cnt = 0
                self._cc_tiles.append(owner)
            sem = owner.sem
        else:
            sem = self._sem_for(owner)
        owner.cnt += inc
        self.ops[eng].append(("dma", waits, fn, (sem, inc)))
        self._mark(("dma", sem, owner.cnt), reads, writes)

    def wait_all(self, eng, tiles):
        waits = self._deps(eng, tiles, (), True)
        self.ops[eng].append(("wait", waits, None, None))

    def emit(self):
        nc = self.nc
        engobj = {"pe": nc.tensor, "act": nc.scalar, "dve": nc.vector, "pool": nc.gpsimd, "sp": nc.sync}
        with nc.Block() as block:
            def run(eng):
                e = engobj[eng]
                for kind, waits, fns, extra in self.ops[eng]:
                    for sem, val in waits:
                        e.wait_ge(sem, val)
                    if kind == "op":
                        n = len(fns)
                        for i, f in enumerate(fns):
                            ins = f()
                            if i == n - 1:
                                ins.then_inc(self.esem[eng], 1)
                    elif kind == "dma":
                        fns().then_inc(extra[0], extra[1])

            @block.tensor
            def _(x):
                run("pe")

            @block.scalar
            def _(x):
                run("act")

            @block.vector
            def _(x):
                run("dve")

            @block.gpsimd
            def _(x):
                run("pool")

            @block.sync
            def _(x):
                run("sp")
        self.ops = {e: [] for e in self.ENG}

    def barrier(self):
        sems = []
        for e in self.ENG:
            if self.count[e] > 0:
                sems.append((("eng", e), self.esem[e], self.count[e]))
        for t in self._dma_tiles + self._cc_tiles:
            sems.append((("dma", t.sem), t.sem, t.cnt))
        for e in self.ENG:
            waits = []
            for key, sem, val in sems:
                if self.known[e].get(key, 0) < val:
                    self.known[e][key] = val
                    waits.append((sem, val))
            if waits:
                self.ops[e].append(("wait", waits, None, None))
        for t in self._dma_tiles:
            self.free_sems.append((t.sem, t.cnt))
            t.sem = None
        self._dma_tiles = []


NCORES = 8
D = 4096
SEQ = 8192
T = SEQ // NCORES
NT = T // 128
KC = D // 128
HALO = 16
ALPHA = float((2 * 2) ** 0.25)
LN_EPS = 1e-5
POOL_W = (2, 4, 8, 16)
NE = 32
DFF = 768
CAP = 128


def _mk(f, *a, **k):
    def g():
        try:
            return f(*a, **k)
        except Exception:
            print("FAILED INSTR:", getattr(f, "__name__", f), [getattr(x, "shape", x) for x in a], {n: getattr(v, "shape", v) for n, v in k.items()})
            raise
    return g


class Ctx:
    def __init__(self, nc):
        self.nc = nc
        self.P = Prog(nc)
        P = self.P
        self.banks = [P.ps([128, 512], F32, "bank%d" % i) for i in range(8)]
        self.identf = P.sb([128, 128], F32, "identf")
        self.identb = P.sb([128, 128], BF16, "identb")
        idf, idb = self.identf, self.identb
        P.op("pool", _mk(nc.gpsimd.memset, idf[:], 1.0), writes=[idf])
        P.op("pool", _mk(nc.gpsimd.affine_select, out=idf[:], in_=idf[:], pattern=[[-1, 128]],
                         compare_op=ALU.is_equal, fill=0.0, base=0, channel_multiplier=1), reads=[idf], writes=[idf])
        P.op("dve", _mk(nc.vector.tensor_copy, out=idb[:], in_=idf[:]), reads=[idf], writes=[idb])


def dma_load(cx, eng, dst_tile, dst_ap, src_tile, src_ap):
    e = {"sp": cx.nc.sync, "pool": cx.nc.gpsimd, "act": cx.nc.scalar}[eng]
    cx.P.dma(eng, _mk(e.dma_start, out=dst_ap, in_=src_ap), dst_tile, reads=[src_tile], writes=[dst_tile])


def dma_store(cx, eng, dst_tile, dst_ap, src_tile, src_ap):
    e = {"sp": cx.nc.sync, "pool": cx.nc.gpsimd, "act": cx.nc.scalar}[eng]
    cx.P.dma(eng, _mk(e.dma_start, out=dst_ap, in_=src_ap), src_tile, reads=[src_tile], writes=[dst_tile])


def fm_convert(cx, stk, src, row0, nrows, dstT, col0, evac_engs=("act", "dve")):
    nc, P = cx.nc, cx.P
    stg = [P.sb([128, D], F32, "fmstg", stk) for _ in range(2)]
    stb = [P.sb([128, D], BF16, "fmstb", stk) for _ in range(2)]
    ntile = (nrows + 127) // 128
    for i in range(ntile):
        n = min(128, nrows - i * 128)
        s, b = stg[i % 2], stb[i % 2]
        dma_load(cx, "sp", s, s[0:n, :], src, src[row0 + i * 128: row0 + i * 128 + n, :])
        P.op("act" if i % 2 == 0 else "dve",
             _mk(nc.scalar.copy, out=b[0:n, :], in_=s[0:n, :]) if i % 2 == 0 else
             _mk(nc.vector.tensor_copy, out=b[0:n, :], in_=s[0:n, :]), reads=[s], writes=[b])
        for q in range(4):
            bank = cx.banks[(i * 4 + q) % 8]
            bv = bank[:].bitcast(BF16).rearrange("p (a b) -> p a b", a=8)
            fns = [_mk(nc.tensor.transpose, bv[:, j, 0:n], b[0:n, (q * 8 + j) * 128:(q * 8 + j + 1) * 128], cx.identb[0:n, 0:n])
                   for j in range(8)]
            P.op("pe", fns, reads=[b, cx.identb], writes=[bank])
            c0 = col0 + i * 128
            ev = evac_engs[q % len(evac_engs)]
            if ev == "act":
                P.op("act", _mk(nc.scalar.copy, out=dstT[:, q * 8:(q + 1) * 8, c0:c0 + n], in_=bv[:, :, 0:n]), reads=[bank], writes=[dstT])
            else:
                P.op("dve", _mk(nc.vector.tensor_copy, out=dstT[:, q * 8:(q + 1) * 8, c0:c0 + n], in_=bv[:, :, 0:n]), reads=[bank], writes=[dstT])


class TW:
    def __init__(self, t, col0, cw):
        self.t, self.col0, self.cw = t, col0, cw

    def pick(self, c0):
        return self


class WMulti:
    def __init__(self, parts):
        self.parts = parts

    def pick(self, c0):
        for p in self.parts:
            n = p.t.h.shape[-4] * p.cw
            if p.col0 <= c0 < p.col0 + n:
                return p
        raise KeyError(c0)


def load_w_cols(cx, wt, w, c0, ncols, kc, lead=()):
    if isinstance(w, (TW, WMulti)):
        p = w.pick(c0)
        assert ncols == p.cw and (c0 - p.col0) % p.cw == 0, (c0, ncols, p.col0, p.cw)
        ap = p.t[tuple(lead) + ((c0 - p.col0) // p.cw,)]
        dma_load(cx, "pool", wt, wt[:, 0:kc, 0:ncols], p.t, ap)
    else:
        load_w_fm(cx, wt, w, w[tuple(lead) + (slice(None), slice(c0, c0 + ncols))], kc)


def tile_w(w, cw):
    w = np.asarray(w, np.float32)
    lead = w.shape[:-2]
    K, N = w.shape[-2:]
    v = w.reshape(lead + (K // 128, 128, N // cw, cw))
    nl = len(lead)
    perm = tuple(range(nl)) + (nl + 2, nl + 1, nl + 0, nl + 3)
    return np.ascontiguousarray(v.transpose(perm))


def load_w_fm(cx, wt, w_dram, w_ap, kc):
    n = w_ap.shape[-1]
    dma_load(cx, "pool", wt, wt[:, 0:kc, 0:n], w_dram, w_ap.rearrange("(c p) n -> p c n", p=128))


def stage_pool(cx, xh, w_in, w_group, scale_fm, rfix, w_out, Y):
    nc, P = cx.nc, cx.P
    TH = T + HALO
    with contextlib.ExitStack() as stk:
        xT = P.sb([128, KC, TH], BF16, "xT", stk)
        with contextlib.ExitStack() as stk2:
            pT = P.sb([128, KC, T], BF16, "pT", stk2)
            with contextlib.ExitStack() as stk3:
                fm_convert(cx, stk3, xh, 0, TH, xT, 0)
                P.barrier()
                P.emit()
            wsl = [P.sb([128, KC, 128], BF16, "wsl", stk2) for _ in range(3)]
            ua = [P.sb([128, TH], F32, "ua", stk2) for _ in range(2)]
            ub = [P.sb([128, TH], F32, "ub", stk2) for _ in range(2)]
            uc = [P.sb([128, TH], F32, "uc", stk2) for _ in range(2)]
            sc = P.sb([128, KC], F32, "scale", stk2)
            rf = P.sb([128, 4, 16], F32, "rfix", stk2)
            dma_load(cx, "sp", sc, sc[:], scale_fm, scale_fm[:])
            dma_load(cx, "sp", rf, rf[:], rfix, rfix[:])
            segs = [(0, 512), (512, 512), (1024, TH - 1024)]
            for cc in range(KC):
                wt = wsl[cc % 3]
                load_w_cols(cx, wt, w_in, cc * 128, 128, KC)
                a, b, c = ua[cc % 2], ub[cc % 2], uc[cc % 2]
                for si, (t0, n) in enumerate(segs):
                    bank = cx.banks[(cc * 3 + si) % 8]
                    fns = [_mk(nc.tensor.matmul, bank[:, 0:n], wt[:, k, :], xT[:, k, t0:t0 + n], start=(k == 0), stop=(k == KC - 1))
                           for k in range(KC)]
                    P.op("pe", fns, reads=[wt, xT], writes=[bank])
                    P.op("act", _mk(nc.scalar.copy, out=a[:, t0:t0 + n], in_=bank[:, 0:n]), reads=[bank], writes=[a])
                g = cc // 8
                w = POOL_W[g]
                src, dst = a, b
                sh = 1
                eng = "dve" if cc % 2 == 0 else "pool"
                eo = nc.vector if eng == "dve" else nc.gpsimd
                while sh < w:
                    P.op(eng, _mk(eo.tensor_tensor, out=dst[:, sh:TH], in0=src[:, sh:TH], in1=src[:, 0:TH - sh], op=ALU.add),
                         reads=[src], writes=[dst])
                    src, dst = dst, (c if dst is b else b)
                    sh *= 2
                P.op("dve", _mk(nc.vector.tensor_tensor, out=dst[:, 0:16], in0=src[:, HALO:HALO + 16], in1=rf[:, g, :], op=ALU.mult),
                     reads=[src, rf], writes=[dst])
                P.op("dve", _mk(nc.vector.scalar_tensor_tensor, out=pT[:, cc, 16:T], in0=src[:, HALO + 16:TH], scalar=1.0 / w, in1=a[:, HALO + 16:TH],
                                op0=ALU.mult, op1=ALU.subtract), reads=[src, a], writes=[pT])
                P.op("dve", _mk(nc.vector.tensor_tensor, out=pT[:, cc, 0:16], in0=dst[:, 0:16], in1=a[:, HALO:HALO + 16], op=ALU.subtract),
                     reads=[dst, a], writes=[pT])
            mT = xT
            for g in range(4):
                for dc in range(8):
                    i = g * 8 + dc
                    wt = wsl[i % 3]
                    load_w_cols(cx, wt, w_group, dc * 128, 128, 8, lead=(g,))
                    for si in range(2):
                        bank = cx.banks[(i * 2 + si) % 8]
                        fns = [_mk(nc.tensor.matmul, bank[:, :], wt[:, k, :], pT[:, g * 8 + k, si * 512:(si + 1) * 512], start=(k == 0), stop=(k == 7))
                               for k in range(8)]
                        P.op("pe", fns, reads=[wt, pT], writes=[bank])
                        P.op("act", _mk(nc.scalar.activation, out=mT[:, i, si * 512:(si + 1) * 512], in_=bank[:, :], func=AF.Copy,
                                        scale=sc[:, i:i + 1]), reads=[bank, sc], writes=[mT])
            P.barrier()
            P.emit()
        mT = xT
        wbig = [P.sb([128, KC, 512], BF16, "wbig", stk) for _ in range(2)]
        xin = [P.sb([128, 512], F32, "xin", stk) for _ in range(4)]
        yo = [P.sb([128, 512], F32, "yo", stk) for _ in range(4)]
        for cc in range(D // 512):
            wt = wbig[cc % 2]
            load_w_cols(cx, wt, w_out, cc * 512, 512, KC)
            for tt in range(NT):
                i = cc * NT + tt
                bank = cx.banks[i % 8]
                xi, y = xin[i % 4], yo[i % 4]
                dma_load(cx, "sp", xi, xi[:], xh, xh[HALO + tt * 128:HALO + (tt + 1) * 128, cc * 512:(cc + 1) * 512])
                fns = [_mk(nc.tensor.matmul, bank[:, :], mT[:, k, tt * 128:(tt + 1) * 128], wt[:, k, :], start=(k == 0), stop=(k == KC - 1))
                       for k in range(KC)]
                P.op("pe", fns, reads=[wt, mT], writes=[bank])
                P.op("dve", _mk(nc.vector.scalar_tensor_tensor, out=y[:], in0=xi[:], scalar=ALPHA, in1=bank[:, :], op0=ALU.mult, op1=ALU.add),
                     reads=[xi, bank], writes=[y])
                dma_store(cx, "sp", Y, Y[tt * 128:(tt + 1) * 128, cc * 512:(cc + 1) * 512], y, y[:])
        P.barrier()
        P.emit()


LNB = 4


def stage_ln(cx, Y, g_bc, b_bc, X1, X1b=None):
    nc, P = cx.nc, cx.P
    with contextlib.ExitStack() as stk:
        gt = P.sb([128, D], F32, "lng", stk)
        bt = P.sb([128, D], F32, "lnb", stk)
        dma_load(cx, "sp", gt, gt[:], g_bc, g_bc[:])
        dma_load(cx, "sp", bt, bt[:], b_bc, b_bc[:])
        yt = [P.sb([128, D], F32, "lny", stk) for _ in range(LNB)]
        ot = [P.sb([128, D], F32, "lno", stk) for _ in range(LNB)]
        ob = [P.sb([128, D], BF16, "lnob", stk) for _ in range(LNB)]
        st = [P.sb([128, 8, 6], F32, "lnst", stk) for _ in range(LNB)]
        mv = [P.sb([128, 4], F32, "lnmv", stk) for _ in range(LNB)]
        for tt in range(NT):
            y, o, s, m, obf = yt[tt % LNB], ot[tt % LNB], st[tt % LNB], mv[tt % LNB], ob[tt % LNB]
            dma_load(cx, "sp", y, y[:], Y, Y[tt * 128:(tt + 1) * 128, :])
            fns = [_mk(nc.vector.bn_stats, out=s[:, j, :], in_=y[:, j * 512:(j + 1) * 512]) for j in range(8)]
            P.op("dve", fns, reads=[y], writes=[s])
            P.op("dve", _mk(nc.vector.bn_aggr, out=m[:, 0:2], in_=s[:]), reads=[s], writes=[m])
            P.op("dve", _mk(nc.vector.tensor_scalar_add, out=m[:, 1:2], in0=m[:, 1:2], scalar1=LN_EPS), reads=[m], writes=[m])
            P.op("act", _mk(nc.scalar.sqrt, out=m[:, 2:3], in_=m[:, 1:2]), reads=[m], writes=[m])
            P.op("dve", _mk(nc.vector.reciprocal, out=m[:, 2:3], in_=m[:, 2:3]), reads=[m], writes=[m])
            P.op("dve", _mk(nc.vector.scalar_tensor_tensor, out=m[:, 3:4], in0=m[:, 0:1], scalar=-1.0, in1=m[:, 2:3], op0=ALU.mult, op1=ALU.mult),
                 reads=[m], writes=[m])
            P.op("act", _mk(nc.scalar.activation, out=o[:], in_=y[:], func=AF.Identity, bias=m[:, 3:4], scale=m[:, 2:3]), reads=[y, m], writes=[o])
            P.op("pool", _mk(nc.gpsimd.tensor_tensor, out=o[:], in0=o[:], in1=gt[:], op=ALU.mult), reads=[o, gt], writes=[o])
            P.op("dve", _mk(nc.vector.tensor_tensor, out=o[:], in0=o[:], in1=bt[:], op=ALU.add), reads=[o, bt], writes=[o])
            dma_store(cx, "sp", X1, X1[tt * 128:(tt + 1) * 128, :], o, o[:])
            if X1b is not None:
                P.op("act", _mk(nc.scalar.copy, out=obf[:], in_=o[:]), reads=[o], writes=[obf])
                dma_store(cx, "sp", X1b, X1b[tt * 128:(tt + 1) * 128, :], obf, obf[:])
        P.barrier()
        P.emit()


def stage_moe(cx, X1, X1b, w_router_d, rb_bc, offs_d, tokid_d, w_in_e, w_out_e, YB, Y, dbg=None, stop_after=None):
    nc, P = cx.nc, cx.P
    with contextlib.ExitStack() as stk:
        A_all = P.sb([128, NT, NE], F32, "A_all", stk)
        A_bf = P.sb([128, NT, NE], BF16, "A_bf", stk)
        gate_all = P.sb([128, NT, NE], F32, "gate_all", stk)
        rank_all = P.sb([128, NT, NE], F32, "rank_all", stk)
        idx_i = P.sb([128, NE], I32, "idx_i", stk)
        dsti = P.sb([128, NT, 4], I32, "dsti", stk)
        g12 = P.sb([128, NT, 2], F32, "g12", stk)
        rb = P.sb([128, NE], F32, "rb", stk)
        offs = P.sb([128, NE], F32, "offs", stk)
        tokid = P.sb([128, NT, 2], BF16, "tokid", stk)
        tokidf = P.sb([128, NT, 2], F32, "tokidf", stk)
        iota_f = P.sb([128, 128], F32, "iota_f", stk)
        ones_bf = P.sb([128, 128], BF16, "ones_bf", stk)
        ustr_f = P.sb([128, 128], F32, "ustr_f", stk)
        ustr_bf = P.sb([128, 128], BF16, "ustr_bf", stk)
        dma_load(cx, "sp", rb, rb[:], rb_bc, rb_bc[:])
        dma_load(cx, "sp", offs, offs[:], offs_d, offs_d[:])
        dma_load(cx, "sp", tokidf, tokidf[:], tokid_d, tokid_d[:])
        P.op("dve", _mk(nc.vector.tensor_copy, out=tokid[:], in_=tokidf[:]), reads=[tokidf], writes=[tokid])
        P.op("pool", _mk(nc.gpsimd.iota, iota_f[:], pattern=[[1, 128]], base=0, channel_multiplier=0, allow_small_or_imprecise_dtypes=True), writes=[iota_f])
        P.op("pool", _mk(nc.gpsimd.memset, ones_bf[:], 1.0), writes=[ones_bf])
        P.op("pool", _mk(nc.gpsimd.memset, ustr_f[:], 1.0), writes=[ustr_f])
        P.op("pool", _mk(nc.gpsimd.affine_select, out=ustr_f[:], in_=ustr_f[:], pattern=[[1, 128]], compare_op=ALU.is_ge, fill=0.0,
                         base=-1, channel_multiplier=-1), reads=[ustr_f], writes=[ustr_f])
        P.op("dve", _mk(nc.vector.tensor_copy, out=ustr_bf[:], in_=ustr_f[:]), reads=[ustr_f], writes=[ustr_bf])
        with contextlib.ExitStack() as stk2:
            wr = P.sb([128, KC, NE], F32, "wr", stk2)
            dma_load(cx, "sp", wr, wr[:], w_router_d, w_router_d[:].rearrange("(c p) n -> p c n", p=128))
            xt = [P.sb([128, D], F32, "rx", stk2) for _ in range(2)]
            xTf = [P.sb([128, KC, 128], F32, "rxT", stk2) for _ in range(2)]
            sm = [P.sb([128, NT, NE], F32, "rs%d" % j, stk2) for j in range(6)]
            sg = [P.sb([128, NT, 8], F32, "rg%d" % j, stk2) for j in range(4)]
            sd = P.sb([128, NT, 2], F32, "rden", stk2)
            sc, sel, eq, sel2, ge, gsel = sm
            m1, m2, gs, ohg = sg
            gm = P.sb([128, NT], F32, "rgm", stk2)
            for tt in range(NT):
                x, xT = xt[tt % 2], xTf[tt % 2]
                dma_load(cx, "sp", x, x[:], X1, X1[tt * 128:(tt + 1) * 128, :])
                for q in range(8):
                    bank = cx.banks[q % 4]
                    fns = [_mk(nc.tensor.transpose, bank[:, j * 128:(j + 1) * 128], x[:, (q * 4 + j) * 128:(q * 4 + j + 1) * 128], cx.identf[:])
                           for j in range(4)]
                    P.op("pe", fns, reads=[x, cx.identf], writes=[bank])
                    if q % 2 == 0:
                        P.op("act", _mk(nc.scalar.copy, out=xT[:, q * 4:(q + 1) * 4, :], in_=bank[:].rearrange("p (a b) -> p a b", a=4)), reads=[bank], writes=[xT])
                    else:
                        P.op("dve", _mk(nc.vector.tensor_copy, out=xT[:, q * 4:(q + 1) * 4, :], in_=bank[:].rearrange("p (a b) -> p a b", a=4)), reads=[bank], writes=[xT])
                lb = cx.banks[4 + tt % 2]
                fns = [_mk(nc.tensor.matmul, lb[:, 0:NE], xT[:, k, :], wr[:, k, :], start=(k == 0), stop=(k == KC - 1)) for k in range(KC)]
                P.op("pe", fns, reads=[xT, wr], writes=[lb])
                P.op("act", _mk(nc.scalar.activation, out=sc[:, tt, :], in_=lb[:, 0:NE], func=AF.Sigmoid), reads=[lb], writes=[sc])
            V = nc.vector
            g4 = lambda t: t[:].rearrange("p t (g k) -> p t g k", k=4)
            b4 = lambda t: t[:].unsqueeze(3).to_broadcast([128, NT, 8, 4])
            P.op("dve", _mk(V.tensor_tensor, out=sel[:], in0=sc[:], in1=rb[:].unsqueeze(1).to_broadcast([128, NT, NE]), op=ALU.add), reads=[sc, rb], writes=[sel])
            P.op("dve", _mk(V.tensor_reduce, out=m1[:], in_=g4(sel), axis=AX.X, op=ALU.max), reads=[sel], writes=[m1])
            P.op("dve", _mk(V.tensor_tensor, out=g4(eq), in0=g4(sel), in1=b4(m1), op=ALU.is_equal), reads=[sel, m1], writes=[eq])
            P.op("dve", _mk(V.scalar_tensor_tensor, out=sel2[:], in0=eq[:], scalar=-1e9, in1=sel[:], op0=ALU.mult, op1=ALU.add), reads=[eq, sel], writes=[sel2])
            P.op("dve", _mk(V.tensor_reduce, out=m2[:], in_=g4(sel2), axis=AX.X, op=ALU.max), reads=[sel2], writes=[m2])
            P.op("dve", _mk(V.tensor_tensor, out=gs[:], in0=m1[:], in1=m2[:], op=ALU.add), reads=[m1, m2], writes=[gs])
            P.op("dve", _mk(V.tensor_reduce, out=gm[:], in_=gs[:], axis=AX.X, op=ALU.max), reads=[gs], writes=[gm])
            P.op("dve", _mk(V.tensor_tensor, out=ohg[:], in0=gs[:], in1=gm[:].unsqueeze(2).to_broadcast([128, NT, 8]), op=ALU.is_equal), reads=[gs, gm], writes=[ohg])
            P.op("dve", _mk(V.tensor_tensor, out=g4(ge), in0=g4(sel), in1=b4(m2), op=ALU.is_ge), reads=[sel, m2], writes=[ge])
            P.op("dve", _mk(V.tensor_tensor, out=g4(A_all), in0=g4(ge), in1=b4(ohg), op=ALU.mult), reads=[ge, ohg], writes=[A_all])
            P.op("dve", _mk(V.tensor_copy, out=A_bf[:], in_=A_all[:]), reads=[A_all], writes=[A_bf])
            P.op("dve", _mk(V.tensor_tensor, out=gsel[:], in0=A_all[:], in1=sc[:], op=ALU.mult), reads=[A_all, sc], writes=[gsel])
            P.op("dve", _mk(V.tensor_reduce, out=sd[:, :, 0], in_=gsel[:], axis=AX.X, op=ALU.add), reads=[gsel], writes=[sd])
            P.op("dve", _mk(V.reciprocal, out=sd[:, :, 1], in_=sd[:, :, 0]), reads=[sd], writes=[sd])
            P.op("dve", _mk(V.tensor_tensor, out=gate_all[:], in0=gsel[:], in1=sd[:, :, 1:2].to_broadcast([128, NT, NE]), op=ALU.mult),
                 reads=[gsel, sd], writes=[gate_all])
            rbk = cx.banks[6]
            fns = []
            for tt in range(NT):
                for i in range(tt + 1):
                    fns.append(_mk(nc.tensor.matmul, rbk[:, tt * NE:(tt + 1) * NE], (ustr_bf if i == tt else ones_bf)[:], A_bf[:, i, :],
                                   start=(i == 0), stop=(i == tt)))
            P.op("pe", fns, reads=[A_bf, ustr_bf, ones_bf], writes=[rbk])
            P.op("act", _mk(nc.scalar.copy, out=rank_all[:].rearrange("p a b -> p (a b)"), in_=rbk[:, 0:NT * NE]), reads=[rbk], writes=[rank_all])
            vt = P.sb([128, NT, NE], F32, "vt", stk2)
            vm = P.sb([128, NT, NE], F32, "vm", stk2)
            fs = P.sb([128, NT, 4], F32, "fs", stk2)
            V = nc.vector
            P.op("dve", _mk(V.tensor_tensor, out=vt[:], in0=rank_all[:], in1=offs[:].unsqueeze(1).to_broadcast([128, NT, NE]), op=ALU.add),
                 reads=[rank_all, offs], writes=[vt])
            P.op("dve", _mk(V.tensor_tensor, out=vt[:], in0=vt[:], in1=A_all[:], op=ALU.mult), reads=[vt, A_all], writes=[vt])
            P.op("dve", _mk(V.tensor_reduce, out=fs[:, :, 0], in_=vt[:], axis=AX.X, op=ALU.max), reads=[vt], writes=[fs])
            P.op("dve", _mk(V.tensor_reduce, out=fs[:, :, 1], in_=vt[:], axis=AX.X, op=ALU.add), reads=[vt], writes=[fs])
            P.op("dve", _mk(V.tensor_tensor, out=fs[:, :, 1], in0=fs[:, :, 1], in1=fs[:, :, 0], op=ALU.subtract), reads=[fs], writes=[fs])
            P.op("dve", _mk(V.tensor_tensor, out=vm[:], in0=vt[:], in1=fs[:, :, 0:1].to_broadcast([128, NT, NE]), op=ALU.is_equal),
                 reads=[vt, fs], writes=[vm])
            P.op("dve", _mk(V.tensor_tensor, out=vm[:], in0=vm[:], in1=gate_all[:], op=ALU.mult), reads=[vm, gate_all], writes=[vm])
            P.op("dve", _mk(V.tensor_reduce, out=g12[:, :, 0], in_=vm[:], axis=AX.X, op=ALU.add), reads=[vm], writes=[g12])
            P.op("dve", _mk(V.tensor_reduce, out=fs[:, :, 2], in_=gate_all[:], axis=AX.X, op=ALU.add), reads=[gate_all], writes=[fs])
            P.op("dve", _mk(V.tensor_tensor, out=g12[:, :, 1], in0=fs[:, :, 2], in1=g12[:, :, 0], op=ALU.subtract), reads=[fs, g12], writes=[g12])
            P.op("dve", _mk(V.tensor_scalar, out=fs[:, :, 0:2], in0=fs[:, :, 0:2], scalar1=-1.0, scalar2=float(NE * CAP - 1), op0=ALU.add, op1=ALU.min),
                 reads=[fs], writes=[fs])
            P.op("dve", _mk(V.tensor_scalar_max, out=fs[:, :, 0:2], in0=fs[:, :, 0:2], scalar1=0.0), reads=[fs], writes=[fs])
            fs2 = P.sb([128, NT, 4], F32, "fs2", stk2)
            for k in range(2):
                P.op("dve", _mk(V.tensor_scalar, out=fs2[:, :, 2 * k:2 * k + 1], in0=fs[:, :, k:k + 1], scalar1=2.0, scalar2=None, op0=ALU.mult), reads=[fs], writes=[fs2])
                P.op("dve", _mk(V.tensor_scalar, out=fs2[:, :, 2 * k + 1:2 * k + 2], in0=fs[:, :, k:k + 1], scalar1=2.0, scalar2=1.0, op0=ALU.mult, op1=ALU.add),
                     reads=[fs], writes=[fs2])
            P.op("dve", _mk(V.tensor_copy, out=dsti[:], in_=fs2[:]), reads=[fs2], writes=[dsti])
            pe_t = [P.sb([128, NT, 128], BF16, "pe_t", stk2) for _ in range(2)]
            ibk = cx.banks[7]
            for e in range(NE):
                pt = pe_t[e % 2]
                for tt in range(NT):
                    P.op("dve", _mk(V.tensor_scalar, out=pt[:, tt, :], in0=iota_f[:], scalar1=rank_all[:, tt, e:e + 1], scalar2=A_all[:, tt, e:e + 1],
                                    op0=ALU.is_equal, op1=ALU.mult), reads=[iota_f, rank_all, A_all], writes=[pt])
                fns = [_mk(nc.tensor.matmul, ibk[:, e * 2:(e + 1) * 2], pt[:, tt, :], tokid[:, tt, :], start=(tt == 0), stop=(tt == NT - 1)) for tt in range(NT)]
                P.op("pe", fns, reads=[pt, tokid], writes=[ibk])
            idxf = P.sb([128, NE, 2], F32, "idxf", stk2)
            idx1 = P.sb([128, NE], F32, "idx1", stk2)
            P.op("act", _mk(nc.scalar.copy, out=idxf[:].rearrange("p a b -> p (a b)"), in_=ibk[:, 0:2 * NE]), reads=[ibk], writes=[idxf])
            P.op("dve", _mk(V.scalar_tensor_tensor, out=idx1[:], in0=idxf[:, :, 1], scalar=128.0, in1=idxf[:, :, 0], op0=ALU.mult, op1=ALU.add),
                 reads=[idxf], writes=[idx1])
            P.op("dve", _mk(V.tensor_copy, out=idx_i[:], in_=idx1[:]), reads=[idx1], writes=[idx_i])
            if dbg is not None:
                for nm, t in (("A_all", A_all), ("gate_all", gate_all), ("rank_all", rank_all), ("idx_i", idx_i), ("dsti", dsti), ("g12", g12)):
                    dma_store(cx, "sp", dbg[nm], dbg[nm][:], t, t[:])
            P.barrier()
            P.emit()
        if stop_after == "router":
            return
        with contextlib.ExitStack() as stk2:
            xg = [P.sb([128, D], BF16, "xg", stk2) for _ in range(2)]
            xgT = [P.sb([128, KC, 128], BF16, "xgT", stk2) for _ in range(2)]
            wt_s = [P.sb([128, KC, 256], BF16, "wie", stk2) for _ in range(3)]
            wo_s = [P.sb([128, 6, 1024], BF16, "woe", stk2) for _ in range(3)]
            hs = [P.sb([128, 2 * DFF], F32, "hs", stk2) for _ in range(2)]
            act_b = [P.sb([128, DFF], BF16, "actb", stk2) for _ in range(2)]
            actT = [P.sb([128, 6, 128], BF16, "actT", stk2) for _ in range(2)]
            ybs = [P.sb([128, D], F32, "ybs", stk2) for _ in range(2)]
            wi_n = 0
            wo_n = 0
            for e in range(NE):
                g, gT, h, ab, aT, yb = xg[e % 2], xgT[e % 2], hs[e % 2], act_b[e % 2], actT[e % 2], ybs[e % 2]
                P.dma("pool", _mk(nc.gpsimd.indirect_dma_start, out=g[:], out_offset=None, in_=X1b[:, :],
                                  in_offset=bass.IndirectOffsetOnAxis(ap=idx_i[:, e:e + 1], axis=0)),
                      g, reads=[X1b, idx_i], writes=[g])
                for q in range(4):
                    bank = cx.banks[q]
                    bv = bank[:].bitcast(BF16).rearrange("p (a b) -> p a b", a=8)
                    fns = [_mk(nc.tensor.transpose, bv[:, j, :], g[:, (q * 8 + j) * 128:(q * 8 + j + 1) * 128], cx.identb[:]) for j in range(8)]
                    P.op("pe", fns, reads=[g, cx.identb], writes=[bank])
                    if q % 2 == 0:
                        P.op("act", _mk(nc.scalar.copy, out=gT[:, q * 8:(q + 1) * 8, :], in_=bv), reads=[bank], writes=[gT])
                    else:
                        P.op("dve", _mk(nc.vector.tensor_copy, out=gT[:, q * 8:(q + 1) * 8, :], in_=bv), reads=[bank], writes=[gT])
                for c in range(6):
                    wt = wt_s[wi_n % 3]
                    wi_n += 1
                    load_w_cols(cx, wt, w_in_e, c * 256, 256, KC, lead=(e,))
                    bank = cx.banks[4 + c % 2]
                    fns = [_mk(nc.tensor.matmul, bank[:, 0:256], gT[:, k, :], wt[:, k, :], start=(k == 0), stop=(k == KC - 1)) for k in range(KC)]
                    P.op("pe", fns, reads=[gT, wt], writes=[bank])
                    if c < 3:
                        P.op("act", _mk(nc.scalar.activation, out=h[:, c * 256:(c + 1) * 256], in_=bank[:, 0:256], func=AF.Silu), reads=[bank], writes=[h])
                    else:
                        P.op("act", _mk(nc.scalar.copy, out=h[:, c * 256:(c + 1) * 256], in_=bank[:, 0:256]), reads=[bank], writes=[h])
                P.op("dve", _mk(nc.vector.tensor_tensor, out=ab[:], in0=h[:, 0:DFF], in1=h[:, DFF:2 * DFF], op=ALU.mult), reads=[h], writes=[ab])
                bank = cx.banks[6]
                bv = bank[:].bitcast(BF16).rearrange("p (a b) -> p a b", a=8)
                fns = [_mk(nc.tensor.transpose, bv[:, j, :], ab[:, j * 128:(j + 1) * 128], cx.identb[:]) for j in range(6)]
                P.op("pe", fns, reads=[ab, cx.identb], writes=[bank])
                P.op("dve", _mk(nc.vector.tensor_copy, out=aT[:], in_=bv[:, 0:6, :]), reads=[bank], writes=[aT])
                for c4 in range(4):
                    wo = wo_s[wo_n % 3]
                    wo_n += 1
                    load_w_cols(cx, wo, w_out_e, c4 * 1024, 1024, 6, lead=(e,))
                    for c2 in range(2):
                        c = c4 * 2 + c2
                        bank = cx.banks[c % 4]
                        fns = [_mk(nc.tensor.matmul, bank[:, :], aT[:, k, :], wo[:, k, c2 * 512:(c2 + 1) * 512], start=(k == 0), stop=(k == 5)) for k in range(6)]
                        P.op("pe", fns, reads=[aT, wo], writes=[bank])
                        if c % 2 == 0:
                            P.op("act", _mk(nc.scalar.copy, out=yb[:, c * 512:(c + 1) * 512], in_=bank[:, :]), reads=[bank], writes=[yb])
                        else:
                            P.op("dve", _mk(nc.vector.tensor_copy, out=yb[:, c * 512:(c + 1) * 512], in_=bank[:, :]), reads=[bank], writes=[yb])
                dma_store(cx, "sp", YB, YB[e * CAP:(e + 1) * CAP, :], yb, yb[:])
            P.barrier()
            P.emit()
        if stop_after == "experts":
            return
        with contextlib.ExitStack() as stk2:
            r1 = [P.sb([128, D], F32, "r1", stk2) for _ in range(2)]
            r2 = [P.sb([128, D], F32, "r2", stk2) for _ in range(2)]
            xr = [P.sb([128, D], F32, "xr", stk2) for _ in range(2)]
            for tt in range(NT):
                a, b, x = r1[tt % 2], r2[tt % 2], xr[tt % 2]
                for k, r in ((0, a), (1, b)):
                    for hf in range(2):
                        P.dma("pool", _mk(nc.gpsimd.indirect_dma_start, out=r[:, hf * 2048:(hf + 1) * 2048], out_offset=None,
                                          in_=YB[:, :].rearrange("s (h c) -> (s h) c", h=2),
                                          in_offset=bass.IndirectOffsetOnAxis(ap=dsti[:, tt, 2 * k + hf:2 * k + hf + 1], axis=0)),
                              r, reads=[YB, dsti], writes=[r])
                dma_load(cx, "sp", x, x[:], X1, X1[tt * 128:(tt + 1) * 128, :])
                V = nc.vector
                P.op("act", _mk(nc.scalar.mul, out=x[:], in_=x[:], mul=ALPHA), reads=[x], writes=[x])
                P.op("dve", _mk(V.scalar_tensor_tensor, out=x[:], in0=a[:], scalar=g12[:, tt, 0:1], in1=x[:], op0=ALU.mult, op1=ALU.add),
                     reads=[a, g12, x], writes=[x])
                P.op("dve", _mk(V.scalar_tensor_tensor, out=x[:], in0=b[:], scalar=g12[:, tt, 1:2], in1=x[:], op0=ALU.mult, op1=ALU.add),
                     reads=[b, g12, x], writes=[x])
                dma_store(cx, "sp", Y, Y[tt * 128:(tt + 1) * 128, :], x, x[:])
            P.barrier()
            P.emit()


DI = 8192
NH = 128
NG = 8
HPG = 16
HD = 64
NS = 128
ZOFF, XOFF, BOFF, COFF, DTOFF = 0, DI, 2 * DI, 2 * DI + NG * NS, 2 * DI + 2 * NG * NS
RMS_EPS = 1e-5


def ssd_consts(cx, stk):
    nc, P = cx.nc, cx.P
    c = {}
    c["uincl"] = P.sb([128, 128], F32, "uincl", stk)
    c["onesf"] = P.sb([128, 128], F32, "onesf", stk)
    c["mask"] = P.sb([128, 128], F32, "maskf", stk)
    P.op("pool", _mk(nc.gpsimd.memset, c["onesf"][:], 1.0), writes=[c["onesf"]])
    P.op("pool", _mk(nc.gpsimd.memset, c["uincl"][:], 1.0), writes=[c["uincl"]])
    P.op("pool", _mk(nc.gpsimd.affine_select, out=c["uincl"][:], in_=c["uincl"][:], pattern=[[1, 128]], compare_op=ALU.is_ge, fill=0.0,
                     base=0, channel_multiplier=-1), reads=[c["uincl"]], writes=[c["uincl"]])
    P.op("pool", _mk(nc.gpsimd.memset, c["mask"][:], 0.0), writes=[c["mask"]])
    P.op("pool", _mk(nc.gpsimd.affine_select, out=c["mask"][:], in_=c["mask"][:], pattern=[[1, 128]], compare_op=ALU.is_ge, fill=-1e30,
                     base=0, channel_multiplier=-1), reads=[c["mask"]], writes=[c["mask"]])
    return c


def stage_ssd_a(cx, x2h, w_in, convw_fm, convb_fm, dtb_bc, alog_bc, dsk_bc, YL, SL, TOTC, EAG, CTd, ACTd):
    nc, P = cx.nc, cx.P
    V, G, S = nc.vector, nc.gpsimd, nc.scalar
    TH = T + HALO
    with contextlib.ExitStack() as stk:
        x2T = P.sb([128, KC, TH], BF16, "x2T", stk)
        with contextlib.ExitStack() as stk3:
            fm_convert(cx, stk3, x2h, 0, TH, x2T, 0)
            P.barrier()
            P.emit()
        K = ssd_consts(cx, stk)
        wsl = [P.sb([128, KC, 128], BF16, "wsl", stk) for _ in range(2)]
        cw = P.sb([128, 80, 4], F32, "cw", stk)
        cb = P.sb([128, 80], F32, "cb", stk)
        dtb = P.sb([128, NH], F32, "dtb", stk)
        aneg = P.sb([128, NH], F32, "aneg", stk)
        dsk = P.sb([128, NH], F32, "dsk", stk)
        dma_load(cx, "sp", cw, cw[:], convw_fm, convw_fm[:])
        dma_load(cx, "sp", cb, cb[:], convb_fm, convb_fm[:])
        dma_load(cx, "sp", dtb, dtb[:], dtb_bc, dtb_bc[:])
        dma_load(cx, "sp", aneg, aneg[:], alog_bc, alog_bc[:])
        dma_load(cx, "sp", dsk, dsk[:], dsk_bc, dsk_bc[:])
        P.op("act", _mk(S.activation, out=aneg[:], in_=aneg[:], func=AF.Exp), reads=[aneg], writes=[aneg])
        P.op("dve", _mk(V.tensor_scalar, out=aneg[:], in0=aneg[:], scalar1=-1.0, scalar2=None, op0=ALU.mult), reads=[aneg], writes=[aneg])
        dt = P.sb([128, NT, NH], F32, "dt", stk)
        dtA = P.sb([128, NT, NH], F32, "dtA", stk)
        acum = P.sb([128, NT, NH], F32, "acum", stk)
        eac = P.sb([128, NT, NH], F32, "eac", stk)
        dend = P.sb([128, NT, NH], F32, "dend", stk)
        cdec = P.sb([128, NT, NH], F32, "cdec", stk)
        eag = P.sb([128, NT, NH], F32, "eag", stk)
        run = P.sb([128, NH], F32, "run", stk)
        tmp = [P.sb([128, NH], F32, "dtt%d" % i, stk) for i in range(4)]
        wt = wsl[0]
        load_w_cols(cx, wt, w_in, DTOFF, NH, KC)
        P.op("pool", _mk(G.memset, run[:], 0.0), writes=[run])
        for c in range(NT):
            bank = cx.banks[c % 2]
            fns = [_mk(nc.tensor.matmul, bank[:, 0:NH], x2T[:, k, HALO + c * 128:HALO + (c + 1) * 128], wt[:, k, :], start=(k == 0), stop=(k == KC - 1))
                   for k in range(KC)]
            P.op("pe", fns, reads=[x2T, wt], writes=[bank])
            xr, ax, ex, lx = tmp
            P.op("dve", _mk(V.tensor_tensor, out=xr[:], in0=bank[:, 0:NH], in1=dtb[:], op=ALU.add), reads=[bank, dtb], writes=[xr])
            P.op("act", _mk(S.activation, out=ax[:], in_=xr[:], func=AF.Abs), reads=[xr], writes=[ax])
            P.op("act", _mk(S.activation, out=ex[:], in_=ax[:], func=AF.Exp, scale=-1.0), reads=[ax], writes=[ex])
            P.op("act", _mk(S.activation, out=lx[:], in_=ex[:], func=AF.Ln, bias=1.0, scale=1.0), reads=[ex], writes=[lx])
            P.op("dve", _mk(V.scalar_tensor_tensor, out=dt[:, c, :], in0=xr[:], scalar=0.0, in1=lx[:], op0=ALU.max, op1=ALU.add), reads=[xr, lx], writes=[dt])
            P.op("dve", _mk(V.tensor_tensor, out=dtA[:, c, :], in0=dt[:, c, :], in1=aneg[:], op=ALU.mult), reads=[dt, aneg], writes=[dtA])
            b2 = cx.banks[2 + c % 2]
            P.op("pe", [_mk(nc.tensor.matmul, b2[:, 0:NH], K["uincl"][:], dtA[:, c, :], start=True, stop=True),
                        _mk(nc.tensor.matmul, b2[:, NH:2 * NH], K["onesf"][:], dtA[:, c, :], start=True, stop=True)],
                 reads=[K["uincl"], K["onesf"], dtA], writes=[b2])
            P.op("act", _mk(S.copy, out=acum[:, c, :], in_=b2[:, 0:NH]), reads=[b2], writes=[acum])
            P.op("act", _mk(S.activation, out=eac[:, c, :], in_=b2[:, 0:NH], func=AF.Exp), reads=[b2], writes=[eac])
            P.op("act", _mk(S.activation, out=cdec[:, c, :], in_=b2[:, NH:2 * NH], func=AF.Exp), reads=[b2], writes=[cdec])
            P.op("dve", _mk(V.tensor_tensor, out=ax[:], in0=b2[:, NH:2 * NH], in1=acum[:, c, :], op=ALU.subtract), reads=[b2, acum], writes=[ax])
            P.op("act", _mk(S.activation, out=dend[:, c, :], in_=ax[:], func=AF.Exp), reads=[ax], writes=[dend])
            P.op("dve", _mk(V.tensor_tensor, out=ex[:], in0=acum[:, c, :], in1=run[:], op=ALU.add), reads=[acum, run], writes=[ex])
            P.op("act", _mk(S.activation, out=eag[:, c, :], in_=ex[:], func=AF.Exp), reads=[ex], writes=[eag])
            P.op("dve", _mk(V.tensor_tensor, out=run[:], in0=run[:], in1=b2[:, NH:2 * NH], op=ALU.add), reads=[run, b2], writes=[run])
            b3 = cx.banks[4 + c % 2]
            P.op("pe", _mk(nc.tensor.transpose, b3[:, 0:128], acum[:, c, :], cx.identf[:]), reads=[acum, cx.identf], writes=[b3])
            P.op("dve", _mk(V.tensor_copy, out=lx[:], in_=b3[:, 0:128]), reads=[b3], writes=[lx])
            dma_store(cx, "sp", ACTd, ACTd[:, c * 128:(c + 1) * 128], lx, lx[:])
        dma_store(cx, "sp", TOTC, TOTC[:], run, run[:])
        dma_store(cx, "sp", EAG, EAG[:], eag, eag[:])
        P.barrier()
        P.emit()
        ub = [P.sb([128, TH], F32, "ub", stk) for _ in range(2)]
        acc = [P.sb([128, T], F32, "cacc", stk) for _ in range(2)]
        fT = [P.sb([128, T], BF16, "fT", stk) for _ in range(2)]
        xs_tok = P.sb([128, NT, 1024], BF16, "xs_tok", stk)
        b_tok = P.sb([128, NT, NS], BF16, "b_tok", stk)
        BT = P.sb([128, T], BF16, "BT", stk)
        CT = P.sb([128, T], BF16, "CT", stk)
        state = P.sb([128, 1024], F32, "state", stk)
        state_bf = P.sb([128, 1024], BF16, "state_bf", stk)
        acb = [P.sb([128, 8, 128], F32, "acb", stk) for _ in range(2)]
        Et = [P.sb([128, 8, 128], F32, "Et", stk) for _ in range(2)]
        Mt = [P.sb([128, 8, 128], BF16, "Mt", stk) for _ in range(2)]
        cbT = [P.sb([128, 128], F32, "cbT", stk) for _ in range(2)]
        xs_dt = [P.sb([128, 1024], BF16, "xs_dt", stk) for _ in range(2)]
        xs_dd = [P.sb([128, 1024], BF16, "xs_dd", stk) for _ in range(2)]
        yt = [P.sb([128, 1024], F32, "yt", stk) for _ in range(2)]
        y2 = [P.sb([128, 1024], F32, "y2", stk)] * 2
        segs = [(0, 512), (512, 512), (1024, TH - 1024)]
        nload = [0]

        def proj_chunk(col, j, out_fT):
            wt = wsl[nload[0] % 2]
            u, a = ub[nload[0] % 2], acc[nload[0] % 2]
            nload[0] += 1
            load_w_cols(cx, wt, w_in, col, 128, KC)
            for si, (t0, n) in enumerate(segs):
                bank = cx.banks[si]
                fns = [_mk(nc.tensor.matmul, bank[:, 0:n], wt[:, k, :], x2T[:, k, t0:t0 + n], start=(k == 0), stop=(k == KC - 1)) for k in range(KC)]
                P.op("pe", fns, reads=[wt, x2T], writes=[bank])
                P.op("act", _mk(S.copy, out=u[:, t0:t0 + n], in_=bank[:, 0:n]), reads=[bank], writes=[u])
            P.op("dve", _mk(V.tensor_scalar, out=a[:], in0=u[:, HALO - 3:HALO - 3 + T], scalar1=cw[:, j, 0:1], scalar2=None, op0=ALU.mult), reads=[u, cw], writes=[a])
            for k in range(1, 4):
                P.op("dve", _mk(V.scalar_tensor_tensor, out=a[:], in0=u[:, HALO - 3 + k:HALO - 3 + k + T], scalar=cw[:, j, k:k + 1], in1=a[:],
                                op0=ALU.mult, op1=ALU.add), reads=[u, cw, a], writes=[a])
            P.op("act", _mk(S.activation, out=out_fT[:], in_=a[:], func=AF.Silu, bias=cb[:, j:j + 1], scale=1.0), reads=[a, cb], writes=[out_fT])

        for g in range(NG):
            for j8 in range(8):
                f = fT[j8 % 2]
                proj_chunk(XOFF + g * 1024 + j8 * 128, g * 8 + j8, f)
                bank = cx.banks[3 + j8 % 2]
                bv = bank[:].bitcast(BF16).rearrange("p (a b) -> p a b", a=8)
                fns = [_mk(nc.tensor.transpose, bv[:, c, :], f[:, c * 128:(c + 1) * 128], cx.identb[:]) for c in range(NT)]
                P.op("pe", fns, reads=[f, cx.identb], writes=[bank])
                P.op("dve" if j8 % 2 else "act",
                     _mk(V.tensor_copy, out=xs_tok[:, :, j8 * 128:(j8 + 1) * 128], in_=bv) if j8 % 2 else
                     _mk(S.copy, out=xs_tok[:, :, j8 * 128:(j8 + 1) * 128], in_=bv), reads=[bank], writes=[xs_tok])
            proj_chunk(BOFF + g * NS, 64 + g, BT)
            bank = cx.banks[3]
            bv = bank[:].bitcast(BF16).rearrange("p (a b) -> p a b", a=8)
            fns = [_mk(nc.tensor.transpose, bv[:, c, :], BT[:, c * 128:(c + 1) * 128], cx.identb[:]) for c in range(NT)]
            P.op("pe", fns, reads=[BT, cx.identb], writes=[bank])
            P.op("act", _mk(S.copy, out=b_tok[:], in_=bv), reads=[bank], writes=[b_tok])
            proj_chunk(COFF + g * NS, 72 + g, CT)
            dma_store(cx, "sp", CTd, CTd[g, :, :], CT, CT[:])
            P.op("pool", _mk(G.memset, state[:], 0.0), writes=[state])
            P.op("pool", _mk(G.memset, state_bf[:], 0.0), writes=[state_bf])
            hs = slice(g * HPG, (g + 1) * HPG)
            for c in range(NT):
                i = g * NT + c
                ts = slice(c * 128, (c + 1) * 128)
                xd, xdd, y, yb2, cb_s = xs_dt[i % 2], xs_dd[i % 2], yt[i % 2], y2[i % 2], cbT[i % 2]
                v3 = lambda t: t[:].rearrange("p (h d) -> p h d", d=HD)
                bc_hd = lambda ap: ap.unsqueeze(2).to_broadcast([128, HPG, HD])
                P.op("dve", _mk(V.tensor_tensor, out=v3(xd), in0=xs_tok[:, c, :].rearrange("p (h d) -> p h d", d=HD), in1=bc_hd(dt[:, c, hs]), op=ALU.mult),
                     reads=[xs_tok, dt], writes=[xd])
                P.op("pool", _mk(G.tensor_tensor, out=v3(xdd), in0=v3(xd), in1=bc_hd(dend[:, c, hs]), op=ALU.mult), reads=[xd, dend], writes=[xdd])
                bk0 = cx.banks[0]
                P.op("pe", _mk(nc.tensor.matmul, bk0[:, 0:128], BT[:, ts], CT[:, ts], start=True, stop=True), reads=[BT, CT], writes=[bk0])
                P.op("act", _mk(S.copy, out=cb_s[:], in_=bk0[:, 0:128]), reads=[bk0], writes=[cb_s])
                if c > 0:
                    for hf in range(2):
                        bk = cx.banks[3 + hf]
                        P.op("pe", _mk(nc.tensor.matmul, bk[:, :], CT[:, ts], state_bf[:, hf * 512:(hf + 1) * 512], start=True, stop=True),
                             reads=[CT, state_bf], writes=[bk])
                for hf in range(2):
                    ab, E, M = acb[(2 * i + hf) % 2], Et[(2 * i + hf) % 2], Mt[(2 * i + hf) % 2]
                    h0 = g * HPG + hf * 8
                    P.dma("sp", _mk(nc.sync.dma_start, out=ab[:], in_=ACTd[h0:h0 + 8, ts].partition_broadcast(128)), ab, reads=[ACTd], writes=[ab])
                    P.op("pool", _mk(G.tensor_tensor, out=ab[:], in0=ab[:], in1=K["mask"][:].unsqueeze(1).to_broadcast([128, 8, 128]), op=ALU.add),
                         reads=[ab, K["mask"]], writes=[ab])
                    P.op("dve", _mk(V.tensor_tensor, out=E[:], in0=ab[:], in1=acum[:, c, h0:h0 + 8].unsqueeze(2).to_broadcast([128, 8, 128]), op=ALU.subtract),
                         reads=[ab, acum], writes=[E])
                    P.op("act", _mk(S.activation, out=E[:], in_=E[:], func=AF.Exp), reads=[E], writes=[E])
                    P.op("dve", _mk(V.tensor_tensor, out=M[:], in0=E[:], in1=cb_s[:].unsqueeze(1).to_broadcast([128, 8, 128]), op=ALU.mult),
                         reads=[E, cb_s], writes=[M])
                    bk = cx.banks[1 + hf]
                    fns = [_mk(nc.tensor.matmul, bk[:, hh * HD:(hh + 1) * HD], M[:, hh, :], xd[:, (hf * 8 + hh) * HD:(hf * 8 + hh + 1) * HD], start=True, stop=True)
                           for hh in range(8)]
                    P.op("pe", fns, reads=[M, xd], writes=[bk])
                for hf in range(2):
                    bk = cx.banks[5 + hf]
                    P.op("pe", _mk(nc.tensor.matmul, bk[:, :], b_tok[:, c, :], xdd[:, hf * 512:(hf + 1) * 512], start=True, stop=True), reads=[b_tok, xdd], writes=[bk])
                for hf in range(2):
                    ys = slice(hf * 512, (hf + 1) * 512)
                    hh = slice(g * HPG + hf * 8, g * HPG + hf * 8 + 8)
                    v8 = lambda ap: ap.rearrange("p (h d) -> p h d", d=HD)
                    b8 = lambda ap: ap.unsqueeze(2).to_broadcast([128, 8, HD])
                    P.op("dve", _mk(V.tensor_tensor, out=v8(y[:, ys]), in0=v8(xs_tok[:, c, ys]), in1=b8(dsk[:, hh]), op=ALU.mult), reads=[xs_tok, dsk], writes=[y])
                    P.op("dve", _mk(V.tensor_tensor, out=y[:, ys], in0=y[:, ys], in1=cx.banks[1 + hf][:, :], op=ALU.add), reads=[y, cx.banks[1 + hf]], writes=[y])
                    if c > 0:
                        P.op("dve", _mk(V.tensor_tensor, out=v8(yb2[:, ys]), in0=v8(cx.banks[3 + hf][:, :]), in1=b8(eac[:, c, hh]), op=ALU.mult),
                             reads=[cx.banks[3 + hf], eac], writes=[yb2])
                        P.op("pool", _mk(G.tensor_tensor, out=y[:, ys], in0=y[:, ys], in1=yb2[:, ys], op=ALU.add), reads=[y, yb2], writes=[y])
                dma_store(cx, "sp", YL, YL[ts, g * 1024:(g + 1) * 1024], y, y[:])
                for hf in range(2):
                    ys = slice(hf * 512, (hf + 1) * 512)
                    hh = slice(g * HPG + hf * 8, g * HPG + hf * 8 + 8)
                    v8 = lambda ap: ap.rearrange("p (h d) -> p h d", d=HD)
                    b8 = lambda ap: ap.unsqueeze(2).to_broadcast([128, 8, HD])
                    P.op("dve", _mk(V.tensor_tensor, out=v8(state[:, ys]), in0=v8(state[:, ys]), in1=b8(cdec[:, c, hh]), op=ALU.mult), reads=[state, cdec], writes=[state])
                    P.op("dve", _mk(V.tensor_tensor, out=state[:, ys], in0=state[:, ys], in1=cx.banks[5 + hf][:, :], op=ALU.add), reads=[state, cx.banks[5 + hf]], writes=[state])
                P.op("act", _mk(S.copy, out=state_bf[:], in_=state[:]), reads=[state], writes=[state_bf])
            dma_store(cx, "sp", SL, SL[:, g * 1024:(g + 1) * 1024], state, state[:])
        P.barrier()
        P.emit()


def stage_ssd_b(cx, x2h, w_in, w_out, YL, EAG, CTd, S_all, TOT_all, Wk_d, negm_d, normw_bc, YN, Y):
    nc, P = cx.nc, cx.P
    V, G, S = nc.vector, nc.gpsimd, nc.scalar
    TH = T + HALO
    with contextlib.ExitStack() as stk:
        hin_bf = P.sb([128, DI], BF16, "hin_bf", stk)
        eag = P.sb([128, NT, NH], F32, "eagb", stk)
        dma_load(cx, "sp", eag, eag[:], EAG, EAG[:])
        with contextlib.ExitStack() as stk2:
            wk = P.sb([NCORES, NCORES, 128], F32, "wk", stk2)
            tot = P.sb([NCORES, NH], F32, "tot", stk2)
            negm = P.sb([128, NCORES], F32, "negm", stk2)
            coef = P.sb([128, NCORES, NH], F32, "coef", stk2)
            hacc = P.sb([128, DI], F32, "hacc", stk2)
            sj = [P.sb([128, DI], F32, "sj", stk2) for _ in range(2)]
            dma_load(cx, "sp", wk, wk[:], Wk_d, Wk_d[:])
            dma_load(cx, "sp", tot, tot[:], TOT_all, TOT_all[:])
            dma_load(cx, "sp", negm, negm[:], negm_d, negm_d[:])
            for j in range(NCORES):
                bank = cx.banks[j % 2]
                P.op("pe", _mk(nc.tensor.matmul, bank[:, 0:NH], wk[:, j, :], tot[:], start=True, stop=True), reads=[wk, tot], writes=[bank])
                P.op("act", _mk(S.activation, out=coef[:, j, :], in_=bank[:, 0:NH], func=AF.Exp, bias=negm[:, j:j + 1], scale=1.0), reads=[bank, negm], writes=[coef])
                s = sj[j % 2]
                dma_load(cx, "sp", s, s[:], S_all, S_all[j])
                v3 = lambda ap: ap.rearrange("p (h d) -> p h d", d=HD)
                cbc = coef[:, j, :].unsqueeze(2).to_broadcast([128, NH, HD])
                if j == 0:
                    P.op("dve", _mk(V.tensor_tensor, out=v3(hacc[:]), in0=v3(s[:]), in1=cbc, op=ALU.mult), reads=[s, coef], writes=[hacc])
                else:
                    P.op("pool", _mk(G.tensor_tensor, out=v3(s[:]), in0=v3(s[:]), in1=cbc, op=ALU.mult), reads=[s, coef], writes=[s])
                    P.op("dve", _mk(V.tensor_tensor, out=hacc[:], in0=hacc[:], in1=s[:], op=ALU.add), reads=[hacc, s], writes=[hacc])
            P.op("act", _mk(S.copy, out=hin_bf[:], in_=hacc[:]), reads=[hacc], writes=[hin_bf])
            P.barrier()
            P.emit()
        with contextlib.ExitStack() as stk2:
            x2T = P.sb([128, KC, TH], BF16, "x2Tb", stk2)
            with contextlib.ExitStack() as stk3:
                fm_convert(cx, stk3, x2h, 0, TH, x2T, 0)
                P.barrier()
                P.emit()
            y_g0 = P.sb([128, NT, 1024], F32, "y_g", stk2)
            y_gt = [Tile(P, y_g0.h, "y_g%d" % i) for i in range(NT)]
            CT = [P.sb([128, T], BF16, "CTb", stk2) for _ in range(2)]
            nw = [P.sb([128, 1024], F32, "nwb", stk2) for _ in range(2)]
            wz = [P.sb([128, KC, 256], BF16, "wz", stk2) for _ in range(3)]
            tmpc = [P.sb([128, 512], F32, "tmpc", stk2) for _ in range(2)]
            sz = [P.sb([128, 256], F32, "sz", stk2) for _ in range(2)]
            sq = P.sb([128, 1024], F32, "sq", stk2)
            ss = [P.sb([128, 4], F32, "ss", stk2) for _ in range(2)]
            ynb = [P.sb([128, 1024], BF16, "ynb", stk2) for _ in range(2)]
            nz = 0
            for g in range(NG):
                ct, nwg = CT[g % 2], nw[g % 2]
                dma_load(cx, "sp", ct, ct[:], CTd, CTd[g, :, :])
                dma_load(cx, "sp", nwg, nwg[:], normw_bc, normw_bc[:, g * 1024:(g + 1) * 1024])
                for tt in range(NT):
                    ts = slice(tt * 128, (tt + 1) * 128)
                    y_g = y_gt[tt]
                    dma_load(cx, "sp", y_g, y_g[:, tt, :], YL, YL[ts, g * 1024:(g + 1) * 1024])
                    for hf in range(2):
                        bank = cx.banks[(tt * 2 + hf) % 4]
                        tc_ = tmpc[(tt * 2 + hf) % 2]
                        hh = slice(g * HPG + hf * 8, g * HPG + hf * 8 + 8)
                        P.op("pe", _mk(nc.tensor.matmul, bank[:, :], ct[:, ts], hin_bf[:, g * 1024 + hf * 512:g * 1024 + (hf + 1) * 512], start=True, stop=True),
                             reads=[ct, hin_bf], writes=[bank])
                        P.op("dve", _mk(V.tensor_tensor, out=tc_[:].rearrange("p (h d) -> p h d", d=HD), in0=bank[:, :].rearrange("p (h d) -> p h d", d=HD),
                                        in1=eag[:, tt, hh].unsqueeze(2).to_broadcast([128, 8, HD]), op=ALU.mult), reads=[bank, eag], writes=[tc_])
                        P.op("pool", _mk(G.tensor_tensor, out=y_g[:, tt, hf * 512:(hf + 1) * 512], in0=y_g[:, tt, hf * 512:(hf + 1) * 512], in1=tc_[:], op=ALU.add),
                             reads=[y_g, tc_], writes=[y_g])
                for zc in range(4):
                    w = wz[nz % 3]
                    nz += 1
                    load_w_cols(cx, w, w_in, ZOFF + g * 1024 + zc * 256, 256, KC)
                    for tt in range(NT):
                        y_g = y_gt[tt]
                        bank = cx.banks[4 + tt % 4]
                        s_ = sz[tt % 2]
                        fns = [_mk(nc.tensor.matmul, bank[:, 0:256], x2T[:, k, HALO + tt * 128:HALO + (tt + 1) * 128], w[:, k, :], start=(k == 0), stop=(k == KC - 1))
                               for k in range(KC)]
                        P.op("pe", fns, reads=[x2T, w], writes=[bank])
                        P.op("act", _mk(S.activation, out=s_[:], in_=bank[:, 0:256], func=AF.Silu), reads=[bank], writes=[s_])
                        P.op("dve", _mk(V.tensor_tensor, out=y_g[:, tt, zc * 256:(zc + 1) * 256], in0=y_g[:, tt, zc * 256:(zc + 1) * 256], in1=s_[:], op=ALU.mult),
                             reads=[y_g, s_], writes=[y_g])
                for tt in range(NT):
                    s4, yb = ss[tt % 2], ynb[tt % 2]
                    y_g = y_gt[tt]
                    P.op("act", _mk(S.activation, out=sq[:], in_=y_g[:, tt, :], func=AF.Square), reads=[y_g], writes=[sq])
                    P.op("dve", _mk(V.tensor_reduce, out=s4[:, 0:1], in_=sq[:], axis=AX.X, op=ALU.add), reads=[sq], writes=[s4])
                    P.op("dve", _mk(V.tensor_scalar, out=s4[:, 1:2], in0=s4[:, 0:1], scalar1=1.0 / 1024.0, scalar2=RMS_EPS, op0=ALU.mult, op1=ALU.add), reads=[s4], writes=[s4])
                    P.op("act", _mk(S.sqrt, out=s4[:, 2:3], in_=s4[:, 1:2]), reads=[s4], writes=[s4])
                    P.op("dve", _mk(V.reciprocal, out=s4[:, 3:4], in_=s4[:, 2:3]), reads=[s4], writes=[s4])
                    P.op("dve", _mk(V.scalar_tensor_tensor, out=yb[:], in0=y_g[:, tt, :], scalar=s4[:, 3:4], in1=nwg[:], op0=ALU.mult, op1=ALU.mult),
                         reads=[y_g, s4, nwg], writes=[yb])
                    dma_store(cx, "sp", YN, YN[tt * 128:(tt + 1) * 128, g * 1024:(g + 1) * 1024], yb, yb[:])
            P.barrier()
            P.emit()
        with contextlib.ExitStack() as stk2:
            KI = DI // 128
            ynT = P.sb([128, KI, 512], BF16, "ynT", stk2)
            ynt = [P.sb([128, DI], BF16, "ynt", stk2) for _ in range(2)]
            wo = [P.sb([128, KI, 256], BF16, "wo", stk2) for _ in range(2)]
            xin = [P.sb([128, 256], F32, "xinb", stk2) for _ in range(4)]
            yo = [P.sb([128, 256], F32, "yob", stk2) for _ in range(4)]
            nwo = 0
            for half in range(2):
                for t4 in range(4):
                    tt = half * 4 + t4
                    yt_ = ynt[t4 % 2]
                    dma_load(cx, "sp", yt_, yt_[:], YN, YN[tt * 128:(tt + 1) * 128, :])
                    for q in range(8):
                        bank = cx.banks[q]
                        bv = bank[:].bitcast(BF16).rearrange("p (a b) -> p a b", a=8)
                        fns = [_mk(nc.tensor.transpose, bv[:, j, :], yt_[:, (q * 8 + j) * 128:(q * 8 + j + 1) * 128], cx.identb[:]) for j in range(8)]
                        P.op("pe", fns, reads=[yt_, cx.identb], writes=[bank])
                        if q % 2 == 0:
                            P.op("act", _mk(S.copy, out=ynT[:, q * 8:(q + 1) * 8, t4 * 128:(t4 + 1) * 128], in_=bv), reads=[bank], writes=[ynT])
                        else:
                            P.op("dve", _mk(V.tensor_copy, out=ynT[:, q * 8:(q + 1) * 8, t4 * 128:(t4 + 1) * 128], in_=bv), reads=[bank], writes=[ynT])
                for cc in range(D // 256):
                    w = wo[nwo % 2]
                    nwo += 1
                    load_w_cols(cx, w, w_out, cc * 256, 256, KI)
                    for t4 in range(4):
                        tt = half * 4 + t4
                        i = cc * 4 + t4
                        bank = cx.banks[i % 8]
                        xi, y = xin[i % 4], yo[i % 4]
                        dma_load(cx, "sp", xi, xi[:], x2h, x2h[HALO + tt * 128:HALO + (tt + 1) * 128, cc * 256:(cc + 1) * 256])
                        fns = [_mk(nc.tensor.matmul, bank[:, 0:256], ynT[:, k, t4 * 128:(t4 + 1) * 128], w[:, k, :], start=(k == 0), stop=(k == KI - 1)) for k in range(KI)]
                        P.op("pe", fns, reads=[ynT, w], writes=[bank])
                        P.op("dve", _mk(V.scalar_tensor_tensor, out=y[:], in0=xi[:], scalar=ALPHA, in1=bank[:, 0:256], op0=ALU.mult, op1=ALU.add),
                             reads=[xi, bank], writes=[y])
                        dma_store(cx, "sp", Y, Y[tt * 128:(tt + 1) * 128, cc * 256:(cc + 1) * 256], y, y[:])
            P.barrier()
            P.emit()


def _moe_io(P, sfx=""):
    return dict(wr=_din(P, "w_router", [D, NE]), rb=_din(P, "rb_bc", [128, NE]), offs=_din(P, "offs", [128, NE]), tokid=_din(P, "tokid", [128, NT, 2]),
                wie=_din(P, "w_in_e", [NE, D, 2 * DFF]), woe=_din(P, "w_out_e", [NE, DFF, D]),
                g1=_din(P, "g1_bc", [128, D]), b1=_din(P, "b1_bc", [128, D]), g2=_din(P, "g2_bc", [128, D]), b2=_din(P, "b2_bc", [128, D]))


def _post_mixer(cx, Y, io, XOUT):
    P = cx.P
    X1 = P.dram("X1s", [T, D], F32)
    X1b = P.dram("X1bs", [T, D], BF16)
    YB = P.dram("YBs", [NE * CAP, D], F32)
    Y2 = P.dram("Y2s", [T, D], F32)
    stage_ln(cx, Y, io["g1"], io["b1"], X1, X1b)
    stage_moe(cx, X1, X1b, io["wr"], io["rb"], io["offs"], io["tokid"], io["wie"], io["woe"], YB, Y2)
    stage_ln(cx, Y2, io["g2"], io["b2"], XOUT)


def _din(P, name, shape, dt=F32):
    return P.dram(name, shape, dt, kind="ExternalInput")


def _dout(P, name, shape, dt=F32):
    return P.dram(name, shape, dt, kind="ExternalOutput")


FUSED = False


def all_gather(cx, src, dst):
    nc = cx.nc
    cx.P.dma("pool", _mk(nc.gpsimd.collective_compute, "AllGather", ALU.bypass, replica_groups=[list(range(NCORES))],
                         ins=[src[:, :]], outs=[dst[:, :]]), dst, reads=[src], writes=[dst], inc=1)


_WSPEC = {
    "p_w_in": ([D // 128, 128, KC, 128], 0, 128),
    "p_w_group": ([4, 8, 128, 8, 128], 0, 128),
    "p_w_out": ([D // 512, 128, KC, 512], 0, 512),
    "w_in_e0": ([NE, 6, 128, KC, 256], 0, 256), "w_in_e1": ([NE, 6, 128, KC, 256], 0, 256),
    "w_out_e0": ([NE, 4, 128, 6, 1024], 0, 1024), "w_out_e1": ([NE, 4, 128, 6, 1024], 0, 1024),
    "s_w_z": ([DI // 256, 128, KC, 256], 0, 256),
    "s_w_x": ([81, 128, KC, 128], DI, 128),
    "s_w_out": ([D // 256, 128, DI // 128, 256], 0, 256),
}


def _tw(P, name):
    shape, col0, cw = _WSPEC[name]
    return TW(_din(P, name, shape), col0, cw)


def _host_w(inp, names):
    f = lambda a: np.asarray(a, np.float32)
    out = {}
    for n in names:
        if n == "p_w_in":
            out[n] = tile_w(f(inp["pool_w_in"][0]), 128)
        elif n == "p_w_group":
            out[n] = tile_w(f(inp["pool_w_group"][0]), 128)
        elif n == "p_w_out":
            out[n] = tile_w(f(inp["pool_w_out"][0]), 512)
        elif n.startswith("w_in_e"):
            out[n] = tile_w(f(inp["moe_w_in"][int(n[-1])]), 256)
        elif n.startswith("w_out_e"):
            out[n] = tile_w(f(inp["moe_w_out"][int(n[-1])]), 1024)
        elif n == "s_w_z":
            out[n] = tile_w(f(inp["ssd_w_in"][0])[:, :DI], 256)
        elif n == "s_w_x":
            out[n] = tile_w(f(inp["ssd_w_in"][0])[:, DI:], 128)
        elif n == "s_w_out":
            out[n] = tile_w(f(inp["ssd_w_out"][0]), 256)
    return out


def _small_inputs(P):
    return dict(wr=_din(P, "w_router", [D, NE]), rb=_din(P, "rb_bc", [128, NE]), offs=_din(P, "offs", [128, NE]), tokid=_din(P, "tokid", [128, NT, 2]),
                lnp=_din(P, "ln_bc", [8, 128, D]))


def _ssd_small(P):
    return dict(cw=_din(P, "convw_fm", [128, 80, 4]), cb=_din(P, "convb_fm", [128, 80]), dtb=_din(P, "dtb_bc", [128, NH]), al=_din(P, "alog_bc", [128, NH]),
                ds=_din(P, "dsk_bc", [128, NH]))


def _layer0(cx, xh, sm, W, Y, X1, X1b, YB, X2dst):
    P = cx.P
    p_sc = _din(P, "p_scale_fm", [128, KC])
    p_rf = _din(P, "p_rfix", [128, 4, 16])
    stage_pool(cx, xh, W["p_w_in"], W["p_w_group"], p_sc, p_rf, W["p_w_out"], Y)
    stage_ln(cx, Y, _LnView(sm["lnp"], 0), _LnView(sm["lnp"], 1), X1, X1b)
    stage_moe(cx, X1, X1b, sm["wr"], sm["rb"], sm["offs"], sm["tokid"], W["w_in_e0"], W["w_out_e0"], YB, Y)
    stage_ln(cx, Y, _LnView(sm["lnp"], 2), _LnView(sm["lnp"], 3), X2dst)


def _layer1_tail(cx, sm, W, Y, X1, X1b, YB, OUT):
    stage_ln(cx, Y, _LnView(sm["lnp"], 4), _LnView(sm["lnp"], 5), X1, X1b)
    stage_moe(cx, X1, X1b, sm["wr"], sm["rb"], sm["offs"], sm["tokid"], W["w_in_e1"], W["w_out_e1"], YB, Y)
    stage_ln(cx, Y, _LnView(sm["lnp"], 6), _LnView(sm["lnp"], 7), OUT)


def build_fused():
    nc = bass.Bass("TRN2", target_bir_lowering=False)
    cx = Ctx(nc)
    P = cx.P
    xh = _din(P, "xh", [T + HALO, D])
    sm = _small_inputs(P)
    ss = _ssd_small(P)
    W = {n: _tw(P, n) for n in _WSPEC}
    s_wi = WMulti([W["s_w_z"], W["s_w_x"]])
    nwb = _din(P, "normw_bc", [128, DI])
    Wk = _din(P, "Wk", [NCORES, NCORES, 128])
    negm = _din(P, "negm", [128, NCORES])
    hidx = _din(P, "halo_idx", [HALO, 1], I32)
    OUT = _dout(P, "OUT", [T, D])
    Y = P.dram("Ys", [T, D], F32)
    X1 = P.dram("X1s", [T, D], F32)
    X1b = P.dram("X1bs", [T, D], BF16)
    YB = P.dram("YBs", [NE * CAP, D], F32)
    X2H = P.dram("X2Hs", [T + HALO, D], F32)
    hb_in = P.dram("hb_in", [HALO, D], F32)
    hb_all = P.dram("hb_all", [NCORES * HALO + HALO, D], F32)
    hb_g = P.dram("hb_g", [NCORES * HALO, D], F32)
    YL = P.dram("YLs", [T, DI], F32)
    SL = P.dram("SLs", [128, DI], F32)
    TOTC = P.dram("TOTCs", [128, NH], F32)
    tot_in = P.dram("tot_in", [1, NH], F32)
    EAG = P.dram("EAGs", [128, NT, NH], F32)
    CTd = P.dram("CTds", [NG, 128, T], BF16)
    ACTd = P.dram("ACTds", [NH, T], F32)
    S_all = P.dram("S_alls", [NCORES * 128, DI], F32)
    TOT_all = P.dram("TOT_alls", [NCORES, NH], F32)
    YN = P.dram("YNs", [T, DI], BF16)
    _layer0(cx, xh, sm, W, Y, X1, X1b, YB, _RowView(X2H, HALO))
    with contextlib.ExitStack() as stk:
        zt = P.sb([HALO, D], F32, "hz", stk)
        ht = P.sb([HALO, D], F32, "ht", stk)
        hi = P.sb([HALO, 1], I32, "hi", stk)
        P.op("dve", _mk(nc.vector.memset, zt[:], 0.0), writes=[zt])
        dma_store(cx, "sp", hb_all, hb_all[NCORES * HALO:NCORES * HALO + HALO, :], zt, zt[:])
        dma_load(cx, "sp", ht, ht[:], X2H, X2H[T:T + HALO, :])
        dma_store(cx, "sp", hb_in, hb_in[:, :], ht, ht[:])
        all_gather(cx, hb_in, hb_g)
        for r0 in range(0, NCORES * HALO, HALO):
            dma_load(cx, "sp", ht, ht[:], hb_g, hb_g[r0:r0 + HALO, :])
            dma_store(cx, "sp", hb_all, hb_all[r0:r0 + HALO, :], ht, ht[:])
        dma_load(cx, "sp", hi, hi[:], hidx, hidx[:])
        P.barrier()
        P.dma("pool", _mk(nc.gpsimd.indirect_dma_start, out=ht[:], out_offset=None, in_=hb_all[:, :],
                          in_offset=bass.IndirectOffsetOnAxis(ap=hi[:, 0:1], axis=0)), ht, reads=[hb_all, hi], writes=[ht])
        dma_store(cx, "sp", X2H, X2H[0:HALO, :], ht, ht[:])
        P.barrier()
        P.emit()
    stage_ssd_a(cx, X2H, s_wi, ss["cw"], ss["cb"], ss["dtb"], ss["al"], ss["ds"], YL, SL, TOTC, EAG, CTd, ACTd)
    with contextlib.ExitStack() as stk:
        tt_ = P.sb([1, NH], F32, "tt_", stk)
        dma_load(cx, "sp", tt_, tt_[:], TOTC, TOTC[0:1, :])
        dma_store(cx, "sp", tot_in, tot_in[:, :], tt_, tt_[:])
        all_gather(cx, SL, S_all)
        all_gather(cx, tot_in, TOT_all)
        P.barrier()
        P.emit()
    stage_ssd_b(cx, X2H, s_wi, W["s_w_out"], YL, EAG, CTd, _S3View(S_all), TOT_all, Wk, negm, nwb, YN, Y)
    _layer1_tail(cx, sm, W, Y, X1, X1b, YB, OUT)
    P.wait_all("sp", [OUT])
    P.emit()
    return nc


def build_l0():
    nc = bass.Bass("TRN2", target_bir_lowering=False)
    cx = Ctx(nc)
    P = cx.P
    xh = _din(P, "xh", [T + HALO, D])
    sm = _small_inputs(P)
    W = {n: _tw(P, n) for n in ("p_w_in", "p_w_group", "p_w_out", "w_in_e0", "w_out_e0")}
    X2 = _dout(P, "X2", [T, D])
    Y = P.dram("Ys", [T, D], F32)
    X1 = P.dram("X1s", [T, D], F32)
    X1b = P.dram("X1bs", [T, D], BF16)
    YB = P.dram("YBs", [NE * CAP, D], F32)
    _layer0(cx, xh, sm, W, Y, X1, X1b, YB, X2)
    P.wait_all("sp", [X2])
    P.emit()
    return nc


def build_ssd_a():
    nc = bass.Bass("TRN2", target_bir_lowering=False)
    cx = Ctx(nc)
    P = cx.P
    x2h = _din(P, "x2h", [T + HALO, D])
    ss = _ssd_small(P)
    s_wi = WMulti([_tw(P, "s_w_x")])
    YL = _dout(P, "YL", [T, DI])
    SL = _dout(P, "SL", [128, DI])
    TOTC = _dout(P, "TOTC", [128, NH])
    EAG = _dout(P, "EAG", [128, NT, NH])
    CTd = _dout(P, "CTd", [NG, 128, T], BF16)
    ACTd = P.dram("ACTd", [NH, T], F32)
    stage_ssd_a(cx, x2h, s_wi, ss["cw"], ss["cb"], ss["dtb"], ss["al"], ss["ds"], YL, SL, TOTC, EAG, CTd, ACTd)
    P.wait_all("sp", [YL, SL, TOTC, EAG, CTd])
    P.emit()
    return nc


def build_l1b():
    nc = bass.Bass("TRN2", target_bir_lowering=False)
    cx = Ctx(nc)
    P = cx.P
    x2h = _din(P, "x2h", [T + HALO, D])
    sm = _small_inputs(P)
    W = {n: _tw(P, n) for n in ("s_w_z", "s_w_out", "w_in_e1", "w_out_e1")}
    YL = _din(P, "YL", [T, DI])
    EAG = _din(P, "EAG", [128, NT, NH])
    CTd = _din(P, "CTd", [NG, 128, T], BF16)
    S_all = _din(P, "S_all", [NCORES, 128, DI])
    TOT_all = _din(P, "TOT_all", [NCORES, NH])
    Wk = _din(P, "Wk", [NCORES, NCORES, 128])
    negm = _din(P, "negm", [128, NCORES])
    nwb = _din(P, "normw_bc", [128, DI])
    OUT = _dout(P, "OUT", [T, D])
    YN = P.dram("YNs", [T, DI], BF16)
    Y = P.dram("Ys", [T, D], F32)
    X1 = P.dram("X1s", [T, D], F32)
    X1b = P.dram("X1bs", [T, D], BF16)
    YB = P.dram("YBs", [NE * CAP, D], F32)
    stage_ssd_b(cx, x2h, WMulti([W["s_w_z"]]), W["s_w_out"], YL, EAG, CTd, S_all, TOT_all, Wk, negm, nwb, YN, Y)
    _layer1_tail(cx, sm, W, Y, X1, X1b, YB, OUT)
    P.wait_all("sp", [OUT])
    P.emit()
    return nc


class _ViewBase:
    def __init__(self, base):
        object.__setattr__(self, "base", base)

    def __getattr__(self, k):
        return getattr(object.__getattribute__(self, "base"), k)

    def __setattr__(self, k, v):
        setattr(object.__getattribute__(self, "base"), k, v)


class _RowView(_ViewBase):
    def __init__(self, base, row0):
        super().__init__(base)
        object.__setattr__(self, "row0", row0)

    def __getitem__(self, key):
        r0 = object.__getattribute__(self, "row0")
        base = object.__getattribute__(self, "base")
        if not isinstance(key, tuple):
            key = (key, slice(None))
        rs = key[0]
        start = (rs.start or 0) + r0
        stop = (rs.stop if rs.stop is not None else T) + r0
        return base[(slice(start, stop),) + tuple(key[1:])]


class _LnView(_ViewBase):
    def __init__(self, base, idx):
        super().__init__(base)
        object.__setattr__(self, "idx", idx)

    def __getitem__(self, key):
        base = object.__getattribute__(self, "base")
        return base[object.__getattribute__(self, "idx")][key]


class _S3View(_ViewBase):
    def __getitem__(self, j):
        base = object.__getattribute__(self, "base")
        return base[j * 128:(j + 1) * 128, :]


def _bc(v):
    v = np.asarray(v, np.float32)
    return np.ascontiguousarray(np.broadcast_to(v, (128,) + v.shape))


def _halo(xfull, c):
    xh = np.zeros((T + HALO, D), np.float32)
    lo = c * T - HALO
    if lo >= 0:
        xh[:] = xfull[lo:lo + T + HALO]
    else:
        xh[HALO:] = xfull[0:T]
    return xh


def _core_consts(k):
    rf = np.zeros((128, 4, 16), np.float32)
    pos = k * T + np.arange(16) + 1
    for gi, w in enumerate(POOL_W):
        rf[:, gi, :] = (1.0 / np.minimum(pos, w))[None, :]
    Wk = np.zeros((NCORES, NCORES, 128), np.float32)
    negm = np.zeros((128, NCORES), np.float32)
    for j in range(NCORES):
        for i in range(NCORES):
            if j < i < k:
                Wk[i, j, :] = 1.0
        if not j < k:
            negm[:, j] = -1e30
    hidx = (np.arange(HALO) + ((k - 1) * HALO if k > 0 else NCORES * HALO)).astype(np.int32).reshape(HALO, 1)
    return rf, Wk, negm, hidx


def kernel(**inp):
    cores = list(range(NCORES))
    f = lambda a: np.ascontiguousarray(a, np.float32)
    x = f(inp["x"])[0]
    tok = np.zeros((128, NT, 2), np.float32)
    tok[:, :, 0] = np.arange(128)[:, None]
    tok[:, :, 1] = np.arange(NT)[None, :]
    conv_w = f(inp["ssd_conv_w"][0])
    conv_b = f(inp["ssd_conv_b"][0])
    lnb = np.stack([_bc(inp[k][l]) for l in range(2) for k in ("ln_mix_g", "ln_mix_b", "ln_ffn_g", "ln_ffn_b")], 0)
    small = {"w_router": f(inp["moe_w_router"]), "rb_bc": _bc(inp["moe_router_bias"]),
             "offs": _bc((np.arange(NE) * CAP + 1).astype(np.float32)), "tokid": tok, "ln_bc": lnb}
    ssd_small = {"convw_fm": np.ascontiguousarray(conv_w.T.reshape(80, 128, 4).transpose(1, 0, 2)),
                 "convb_fm": np.ascontiguousarray(conv_b.reshape(80, 128).T), "dtb_bc": _bc(inp["ssd_dt_bias"][0]), "alog_bc": _bc(inp["ssd_a_log"][0]),
                 "dsk_bc": _bc(inp["ssd_d"][0])}
    pscale = np.ascontiguousarray(f(inp["pool_scale"][0]).reshape(KC, 128).T)
    consts = [_core_consts(k) for k in cores]
    if FUSED:
        shared = dict(small)
        shared.update(ssd_small)
        shared.update(_host_w(inp, list(_WSPEC)))
        shared.update({"p_scale_fm": pscale, "normw_bc": _bc(inp["ssd_norm_w"][0])})
        maps = []
        for k in cores:
            rf, Wk, negm, hidx = consts[k]
            m = dict(shared)
            m.update({"xh": _halo(x, k), "p_rfix": rf, "Wk": Wk, "negm": negm, "halo_idx": hidx})
            maps.append(m)
        r = run_bass_kernel_spmd(build_fused(), maps, core_ids=cores)
        out = np.concatenate([np.asarray(r.results[c]["OUT"]) for c in cores], 0)
        return out[None].astype(np.float32)
    shared = dict(small)
    shared.update(_host_w(inp, ["p_w_in", "p_w_group", "p_w_out", "w_in_e0", "w_out_e0"]))
    shared["p_scale_fm"] = pscale
    maps = []
    for k in cores:
        m = dict(shared)
        m.update({"xh": _halo(x, k), "p_rfix": consts[k][0]})
        maps.append(m)
    r = run_bass_kernel_spmd(build_l0(), maps, core_ids=cores)
    x2 = np.concatenate([np.asarray(r.results[c]["X2"]) for c in cores], 0)
    del r, maps, shared
    shared = dict(ssd_small)
    shared.update(_host_w(inp, ["s_w_x"]))
    x2h = [_halo(x2, c) for c in cores]
    maps = []
    for k in cores:
        m = dict(shared)
        m["x2h"] = x2h[k]
        maps.append(m)
    ra = run_bass_kernel_spmd(build_ssd_a(), maps, core_ids=cores)
    S_all = np.stack([np.asarray(ra.results[c]["SL"]) for c in cores], 0)
    TOT_all = np.stack([np.asarray(ra.results[c]["TOTC"])[0] for c in cores], 0)
    del maps, shared
    shared = dict(small)
    shared.update(_host_w(inp, ["s_w_z", "s_w_out", "w_in_e1", "w_out_e1"]))
    shared.update({"S_all": S_all, "TOT_all": TOT_all, "normw_bc": _bc(inp["ssd_norm_w"][0])})
    maps = []
    for k in cores:
        m = dict(shared)
        o = ra.results[k]
        m.update({"x2h": x2h[k], "YL": np.asarray(o["YL"]), "EAG": np.asarray(o["EAG"]), "CTd": np.asarray(o["CTd"]), "Wk": consts[k][1], "negm": consts[k][2]})
        maps.append(m)
    rb = run_bass_kernel_spmd(build_l1b(), maps, core_ids=cores)
    out = np.concatenate([np.asarray(rb.results[c]["OUT"]) for c in cores], 0)
    return out[None].astype(np.float32)
```

```python
import contextlib
import numpy as np
import concourse.bass as bass
import concourse.mybir as mybir
from concourse.bass_utils import run_bass_kernel_spmd

F32 = mybir.dt.float32
BF16 = mybir.dt.bfloat16
I32 = mybir.dt.int32
AF = mybir.ActivationFunctionType
ALU = mybir.AluOpType
AX = mybir.AxisListType

SAME_ENGINE_SYNC = True


class Tile:
    def __init__(self, prog, handle, name):
        self.prog = prog
        self.h = handle
        self.name = name
        self.last_w = None
        self.readers = []
        self.sem = None
        self.cnt = 0
        self.excl = False
        self.multi = False
        self.writers = []

    def __getitem__(self, k):
        return self.h[k]

    def ap(self):
        return self.h[:] if not hasattr(self.h, "ap") or not callable(getattr(self.h, "ap")) else self.h.ap()


class Prog:
    ENG = ("pe", "act", "dve", "pool", "sp")

    def __init__(self, nc):
        self.nc = nc
        self.stack = contextlib.ExitStack()
        self.ops = {e: [] for e in self.ENG}
        self.count = {e: 0 for e in self.ENG}
        self.known = {e: {} for e in self.ENG}
        self.esem = {}
        for e in self.ENG:
            self.esem[e] = self.stack.enter_context(nc.semaphore("c_" + e))
        self.free_sems = []
        self._dma_tiles = []
        self._cc_tiles = []
        self.nsem = 0
        self.uid = 0

    def sb(self, shape, dtype, name=None, stack=None):
        self.uid += 1
        name = (name or "t") + "_%d" % self.uid
        h = (stack or self.stack).enter_context(self.nc.sbuf_tensor(name, list(shape), dtype))
        return Tile(self, h, name)

    def ps(self, shape, dtype, name=None, stack=None):
        self.uid += 1
        name = (name or "p") + "_%d" % self.uid
        h = (stack or self.stack).enter_context(self.nc.psum_tensor(name, list(shape), dtype))
        t = Tile(self, h, name)
        t.excl = True
        return t

    def dram(self, name, shape, dtype, kind="Internal"):
        h = self.nc.dram_tensor(name, list(shape), dtype, kind=kind)
        t = Tile(self, h, name)
        t.multi = True
        return t

    def _sem_for(self, tile):
        if tile.sem is None:
            if self.free_sems:
                tile.sem, tile.cnt = self.free_sems.pop()
            else:
                self.nsem += 1
                tile.sem = self.stack.enter_context(self.nc.semaphore("d%d" % self.nsem))
                tile.cnt = 0
            self._dma_tiles.append(tile)
        return tile.sem

    def _deps(self, eng, reads, writes, is_dma):
        deps = []
        reads, writes = self._rw(reads, writes)
        for t in reads:
            if t.multi:
                deps.extend(t.writers)
            elif t.last_w is not None:
                deps.append(t.last_w)
        for t in writes:
            if t.last_w is not None and not t.multi:
                deps.append(t.last_w)
            for r in t.readers:
                if r[0] == "eng" and r[1] == eng and not is_dma:
                    continue
                deps.append(r)
        waits = {}
        for d in deps:
            if d[0] == "eng":
                _, e2, k = d
                if e2 == eng and not is_dma and not SAME_ENGINE_SYNC:
                    continue
                key = ("eng", e2)
            else:
                _, sem, k = d
                key = ("dma", sem)
            if self.known[eng].get(key, 0) >= k:
                continue
            if waits.get(key, 0) < k:
                waits[key] = k
        for key, k in waits.items():
            self.known[eng][key] = k
        out = []
        for key, k in waits.items():
            if key[0] == "eng":
                out.append((self.esem[key[1]], k))
            else:
                out.append((key[1], k))
        return out

    @staticmethod
    def _rw(reads, writes):
        r = [t for t in reads if not t.excl]
        w = list(writes) + [t for t in reads if t.excl]
        return r, w

    def _mark(self, token, reads, writes):
        reads, writes = self._rw(reads, writes)
        for t in reads:
            t.readers.append(token)
        for t in writes:
            t.last_w = token
            t.readers = []
            if t.multi:
                t.writers.append(token)

    def op(self, eng, fns, reads=(), writes=()):
        if callable(fns):
            fns = [fns]
        waits = self._deps(eng, reads, writes, False)
        self.count[eng] += 1
        k = self.count[eng]
        self.ops[eng].append(("op", waits, fns, k))
        self._mark(("eng", eng, k), reads, writes)

    def dma(self, eng, fn, owner, reads=(), writes=(), inc=16):
        waits = self._deps(eng, reads, writes, True)
        if inc != 16:
            if owner.sem is None:
                self.nsem += 1
                owner.sem = self.stack.enter_context(self.nc.semaphore("cc%d" % self.nsem))
                owner.cnt = 0
                self._cc_tiles.append(owner)
            sem = owner.sem
        else:
            sem = self._sem_for(owner)
        owner.cnt += inc
        self.ops[eng].append(("dma", waits, fn, (sem, inc)))
        self._mark(("dma", sem, owner.cnt), reads, writes)

    def wait_all(self, eng, tiles):
        waits = self._deps(eng, tiles, (), True)
        self.ops[eng].append(("wait", waits, None, None))

    def emit(self):
        nc = self.nc
        engobj = {"pe": nc.tensor, "act": nc.scalar, "dve": nc.vector, "pool": nc.gpsimd, "sp": nc.sync}
        with nc.Block() as block:
            def run(eng):
                e = engobj[eng]
                for kind, waits, fns, extra in self.ops[eng]:
                    for sem, val in waits:
                        e.wait_ge(sem, val)
                    if kind == "op":
                        n = len(fns)
                        for i, f in enumerate(fns):
                            ins = f()
                            if i == n - 1:
                                ins.then_inc(self.esem[eng], 1)
                    elif kind == "dma":
                        fns().then_inc(extra[0], extra[1])

            @block.tensor
            def _(x):
                run("pe")

            @block.scalar
            def _(x):
                run("act")

            @block.vector
            def _(x):
                run("dve")

            @block.gpsimd
            def _(x):
                run("pool")

            @block.sync
            def _(x):
                run("sp")
        self.ops = {e: [] for e in self.ENG}

    def barrier(self):
        sems = []
        for e in self.ENG:
            if self.count[e] > 0:
                sems.append((("eng", e), self.esem[e], self.count[e]))
        for t in self._dma_tiles + self._cc_tiles:
            sems.append((("dma", t.sem), t.sem, t.cnt))
        for e in self.ENG:
            waits = []
            for key, sem, val in sems:
                if self.known[e].get(key, 0) < val:
                    self.known[e][key] = val
                    waits.append((sem, val))
            if waits:
                self.ops[e].append(("wait", waits, None, None))
        for t in self._dma_tiles:
            self.free_sems.append((t.sem, t.cnt))
            t.sem = None
        self._dma_tiles = []


NCORES = 8
D = 4096
SEQ = 8192
T = SEQ // NCORES
NT = T // 128
KC = D // 128
HALO = 16
ALPHA = float((2 * 2) ** 0.25)
LN_EPS = 1e-5
POOL_W = (2, 4, 8, 16)
NE = 32
DFF = 768
CAP = 128


def _mk(f, *a, **k):
    def g():
        try:
            return f(*a, **k)
        except Exception:
            print("FAILED INSTR:", getattr(f, "__name__", f), [getattr(x, "shape", x) for x in a], {n: getattr(v, "shape", v) for n, v in k.items()})
            raise
    return g


class Ctx:
    def __init__(self, nc):
        self.nc = nc
        self.P = Prog(nc)
        P = self.P
        self.banks = [P.ps([128, 512], F32, "bank%d" % i) for i in range(8)]
        self.identf = P.sb([128, 128], F32, "identf")
        self.identb = P.sb([128, 128], BF16, "identb")
        idf, idb = self.identf, self.identb
        P.op("pool", _mk(nc.gpsimd.memset, idf[:], 1.0), writes=[idf])
        P.op("pool", _mk(nc.gpsimd.affine_select, out=idf[:], in_=idf[:], pattern=[[-1, 128]],
                         compare_op=ALU.is_equal, fill=0.0, base=0, channel_multiplier=1), reads=[idf], writes=[idf])
        P.op("dve", _mk(nc.vector.tensor_copy, out=idb[:], in_=idf[:]), reads=[idf], writes=[idb])


def dma_load(cx, eng, dst_tile, dst_ap, src_tile, src_ap):
    e = {"sp": cx.nc.sync, "pool": cx.nc.gpsimd, "act": cx.nc.scalar}[eng]
    cx.P.dma(eng, _mk(e.dma_start, out=dst_ap, in_=src_ap), dst_tile, reads=[src_tile], writes=[dst_tile])


def dma_store(cx, eng, dst_tile, dst_ap, src_tile, src_ap):
    e = {"sp": cx.nc.sync, "pool": cx.nc.gpsimd, "act": cx.nc.scalar}[eng]
    cx.P.dma(eng, _mk(e.dma_start, out=dst_ap, in_=src_ap), src_tile, reads=[src_tile], writes=[dst_tile])


def fm_convert(cx, stk, src, row0, nrows, dstT, col0, evac_engs=("act", "dve")):
    nc, P = cx.nc, cx.P
    stg = [P.sb([128, D], F32, "fmstg", stk) for _ in range(2)]
    stb = [P.sb([128, D], BF16, "fmstb", stk) for _ in range(2)]
    ntile = (nrows + 127) // 128
    for i in range(ntile):
        n = min(128, nrows - i * 128)
        s, b = stg[i % 2], stb[i % 2]
        dma_load(cx, "sp", s, s[0:n, :], src, src[row0 + i * 128: row0 + i * 128 + n, :])
        P.op("act" if i % 2 == 0 else "dve",
             _mk(nc.scalar.copy, out=b[0:n, :], in_=s[0:n, :]) if i % 2 == 0 else
             _mk(nc.vector.tensor_copy, out=b[0:n, :], in_=s[0:n, :]), reads=[s], writes=[b])
        for q in range(4):
            bank = cx.banks[(i * 4 + q) % 8]
            bv = bank[:].bitcast(BF16).rearrange("p (a b) -> p a b", a=8)
            fns = [_mk(nc.tensor.transpose, bv[:, j, 0:n], b[0:n, (q * 8 + j) * 128:(q * 8 + j + 1) * 128], cx.identb[0:n, 0:n])
                   for j in range(8)]
            P.op("pe", fns, reads=[b, cx.identb], writes=[bank])
            c0 = col0 + i * 128
            ev = evac_engs[q % len(evac_engs)]
            if ev == "act":
                P.op("act", _mk(nc.scalar.copy, out=dstT[:, q * 8:(q + 1) * 8, c0:c0 + n], in_=bv[:, :, 0:n]), reads=[bank], writes=[dstT])
            else:
                P.op("dve", _mk(nc.vector.tensor_copy, out=dstT[:, q * 8:(q + 1) * 8, c0:c0 + n], in_=bv[:, :, 0:n]), reads=[bank], writes=[dstT])


class TW:
    def __init__(self, t, col0, cw):
        self.t, self.col0, self.cw = t, col0, cw

    def pick(self, c0):
        return self


class WMulti:
    def __init__(self, parts):
        self.parts = parts

    def pick(self, c0):
        for p in self.parts:
            n = p.t.h.shape[-4] * p.cw
            if p.col0 <= c0 < p.col0 + n:
                return p
        raise KeyError(c0)


def load_w_cols(cx, wt, w, c0, ncols, kc, lead=()):
    if isinstance(w, (TW, WMulti)):
        p = w.pick(c0)
        assert ncols == p.cw and (c0 - p.col0) % p.cw == 0, (c0, ncols, p.col0, p.cw)
        ap = p.t[tuple(lead) + ((c0 - p.col0) // p.cw,)]
        dma_load(cx, "pool", wt, wt[:, 0:kc, 0:ncols], p.t, ap)
    else:
        load_w_fm(cx, wt, w, w[tuple(lead) + (slice(None), slice(c0, c0 + ncols))], kc)


def tile_w(w, cw):
    w = np.asarray(w, np.float32)
    lead = w.shape[:-2]
    K, N = w.shape[-2:]
    v = w.reshape(lead + (K // 128, 128, N // cw, cw))
    nl = len(lead)
    perm = tuple(range(nl)) + (nl + 2, nl + 1, nl + 0, nl + 3)
    return np.ascontiguousarray(v.transpose(perm))


def load_w_fm(cx, wt, w_dram, w_ap, kc):
    n = w_ap.shape[-1]
    dma_load(cx, "pool", wt, wt[:, 0:kc, 0:n], w_dram, w_ap.rearrange("(c p) n -> p c n", p=128))


def stage_pool(cx, xh, w_in, w_group, scale_fm, rfix, w_out, Y):
    nc, P = cx.nc, cx.P
    TH = T + HALO
    with contextlib.ExitStack() as stk:
        xT = P.sb([128, KC, TH], BF16, "xT", stk)
        with contextlib.ExitStack() as stk2:
            pT = P.sb([128, KC, T], BF16, "pT", stk2)
            with contextlib.ExitStack() as stk3:
                fm_convert(cx, stk3, xh, 0, TH, xT, 0)
                P.barrier()
                P.emit()
            wsl = [P.sb([128, KC, 128], BF16, "wsl", stk2) for _ in range(3)]
            ua = [P.sb([128, TH], F32, "ua", stk2) for _ in range(2)]
            ub = [P.sb([128, TH], F32, "ub", stk2) for _ in range(2)]
            uc = [P.sb([128, TH], F32, "uc", stk2) for _ in range(2)]
            sc = P.sb([128, KC], F32, "scale", stk2)
            rf = P.sb([128, 4, 16], F32, "rfix", stk2)
            dma_load(cx, "sp", sc, sc[:], scale_fm, scale_fm[:])
            dma_load(cx, "sp", rf, rf[:], rfix, rfix[:])
            segs = [(0, 512), (512, 512), (1024, TH - 1024)]
            for cc in range(KC):
                wt = wsl[cc % 3]
                load_w_cols(cx, wt, w_in, cc * 128, 128, KC)
                a, b, c = ua[cc % 2], ub[cc % 2], uc[cc % 2]
                for si, (t0, n) in enumerate(segs):
                    bank = cx.banks[(cc * 3 + si) % 8]
                    fns = [_mk(nc.tensor.matmul, bank[:, 0:n], wt[:, k, :], xT[:, k, t0:t0 + n], start=(k == 0), stop=(k == KC - 1))
                           for k in range(KC)]
                    P.op("pe", fns, reads=[wt, xT], writes=[bank])
                    P.op("act", _mk(nc.scalar.copy, out=a[:, t0:t0 + n], in_=bank[:, 0:n]), reads=[bank], writes=[a])
                g = cc // 8
                w = POOL_W[g]
                src, dst = a, b
                sh = 1
                eng = "dve" if cc % 2 == 0 else "pool"
                eo = nc.vector if eng == "dve" else nc.gpsimd
                while sh < w:
                    P.op(eng, _mk(eo.tensor_tensor, out=dst[:, sh:TH], in0=src[:, sh:TH], in1=src[:, 0:TH - sh], op=ALU.add),
                         reads=[src], writes=[dst])
                    src, dst = dst, (c if dst is b else b)
                    sh *= 2
                P.op("dve", _mk(nc.vector.tensor_tensor, out=dst[:, 0:16], in0=src[:, HALO:HALO + 16], in1=rf[:, g, :], op=ALU.mult),
                     reads=[src, rf], writes=[dst])
                P.op("dve", _mk(nc.vector.scalar_tensor_tensor, out=pT[:, cc, 16:T], in0=src[:, HALO + 16:TH], scalar=1.0 / w, in1=a[:, HALO + 16:TH],
                                op0=ALU.mult, op1=ALU.subtract), reads=[src, a], writes=[pT])
                P.op("dve", _mk(nc.vector.tensor_tensor, out=pT[:, cc, 0:16], in0=dst[:, 0:16], in1=a[:, HALO:HALO + 16], op=ALU.subtract),
                     reads=[dst, a], writes=[pT])
            mT = xT
            for g in range(4):
                for dc in range(8):
                    i = g * 8 + dc
                    wt = wsl[i % 3]
                    load_w_cols(cx, wt, w_group, dc * 128, 128, 8, lead=(g,))
                    for si in range(2):
                        bank = cx.banks[(i * 2 + si) % 8]
                        fns = [_mk(nc.tensor.matmul, bank[:, :], wt[:, k, :], pT[:, g * 8 + k, si * 512:(si + 1) * 512], start=(k == 0), stop=(k == 7))
                               for k in range(8)]
                        P.op("pe", fns, reads=[wt, pT], writes=[bank])
                        P.op("act", _mk(nc.scalar.activation, out=mT[:, i, si * 512:(si + 1) * 512], in_=bank[:, :], func=AF.Copy,
                                        scale=sc[:, i:i + 1]), reads=[bank, sc], writes=[mT])
            P.barrier()
            P.emit()
        mT = xT
        wbig = [P.sb([128, KC, 512], BF16, "wbig", stk) for _ in range(2)]
        xin = [P.sb([128, 512], F32, "xin", stk) for _ in range(4)]
        yo = [P.sb([128, 512], F32, "yo", stk) for _ in range(4)]
        for cc in range(D // 512):
            wt = wbig[cc % 2]
            load_w_cols(cx, wt, w_out, cc * 512, 512, KC)
            for tt in range(NT):
                i = cc * NT + tt
                bank = cx.banks[i % 8]
                xi, y = xin[i % 4], yo[i % 4]
                dma_load(cx, "sp", xi, xi[:], xh, xh[HALO + tt * 128:HALO + (tt + 1) * 128, cc * 512:(cc + 1) * 512])
                fns = [_mk(nc.tensor.matmul, bank[:, :], mT[:, k, tt * 128:(tt + 1) * 128], wt[:, k, :], start=(k == 0), stop=(k == KC - 1))
                       for k in range(KC)]
                P.op("pe", fns, reads=[wt, mT], writes=[bank])
                P.op("dve", _mk(nc.vector.scalar_tensor_tensor, out=y[:], in0=xi[:], scalar=ALPHA, in1=bank[:, :], op0=ALU.mult, op1=ALU.add),
                     reads=[xi, bank], writes=[y])
                dma_store(cx, "sp", Y, Y[tt * 128:(tt + 1) * 128, cc * 512:(cc + 1) * 512], y, y[:])
        P.barrier()
        P.emit()


def stage_ln(cx, Y, g_bc, b_bc, X1, X1b=None):
    nc, P = cx.nc, cx.P
    V = nc.vector
    with contextlib.ExitStack() as stk:
        gt = P.sb([128, D], F32, "lng", stk)
        bt = P.sb([128, D], F32, "lnb", stk)
        dma_load(cx, "sp", gt, gt[:], g_bc, g_bc[:])
        dma_load(cx, "sp", bt, bt[:], b_bc, b_bc[:])
        yt = [P.sb([128, D], F32, "lny", stk) for _ in range(NT)]
        ob = [P.sb([128, D], BF16, "lnob", stk) for _ in range(2)]
        st = [P.sb([128, 8, 6], F32, "lnst", stk) for _ in range(NT)]
        mv = P.sb([128, NT, 2], F32, "lnmv", stk)
        ve = P.sb([128, NT], F32, "lnve", stk)
        sd = P.sb([128, NT], F32, "lnsd", stk)
        rstd = P.sb([128, NT], F32, "lnrs", stk)
        nmr = P.sb([128, NT], F32, "lnnm", stk)
        for tt in range(NT):
            y, s = yt[tt], st[tt]
            dma_load(cx, "sp", y, y[:], Y, Y[tt * 128:(tt + 1) * 128, :])
            fns = [_mk(V.bn_stats, out=s[:, j, :], in_=y[:, j * 512:(j + 1) * 512]) for j in range(8)]
            P.op("dve", fns, reads=[y], writes=[s])
            P.op("dve", _mk(V.bn_aggr, out=mv[:, tt, :], in_=s[:]), reads=[s], writes=[mv])
        P.op("dve", _mk(V.tensor_scalar_add, out=ve[:], in0=mv[:, :, 1], scalar1=LN_EPS), reads=[mv], writes=[ve])
        P.op("act", _mk(nc.scalar.sqrt, out=sd[:], in_=ve[:]), reads=[ve], writes=[sd])
        P.op("dve", _mk(V.reciprocal, out=rstd[:], in_=sd[:]), reads=[sd], writes=[rstd])
        P.op("dve", _mk(V.scalar_tensor_tensor, out=nmr[:], in0=mv[:, :, 0], scalar=-1.0, in1=rstd[:], op0=ALU.mult, op1=ALU.mult),
             reads=[mv, rstd], writes=[nmr])
        for tt in range(NT):
            y, obf = yt[tt], ob[tt % 2]
            P.op("act", _mk(nc.scalar.activation, out=y[:], in_=y[:], func=AF.Identity, bias=nmr[:, tt:tt + 1], scale=rstd[:, tt:tt + 1]),
                 reads=[y, nmr, rstd], writes=[y])
            P.op("pool", _mk(nc.gpsimd.tensor_tensor, out=y[:], in0=y[:], in1=gt[:], op=ALU.mult), reads=[y, gt], writes=[y])
            P.op("dve", _mk(V.tensor_tensor, out=y[:], in0=y[:], in1=bt[:], op=ALU.add), reads=[y, bt], writes=[y])
            dma_store(cx, "sp", X1, X1[tt * 128:(tt + 1) * 128, :], y, y[:])
            if X1b is not None:
                P.op("act", _mk(nc.scalar.copy, out=obf[:], in_=y[:]), reads=[y], writes=[obf])
                dma_store(cx, "sp", X1b, X1b[tt * 128:(tt + 1) * 128, :], obf, obf[:])
        P.barrier()
        P.emit()


def stage_moe(cx, X1, X1b, w_router_d, rb_bc, offs_d, tokid_d, w_in_e, w_out_e, YB, Y, dbg=None, stop_after=None):
    nc, P = cx.nc, cx.P
    with contextlib.ExitStack() as stk:
        A_all = P.sb([128, NT, NE], F32, "A_all", stk)
        A_bf = P.sb([128, NT, NE], BF16, "A_bf", stk)
        gate_all = P.sb([128, NT, NE], F32, "gate_all", stk)
        rank_all = P.sb([128, NT, NE], F32, "rank_all", stk)
        idx_i = P.sb([128, NE], I32, "idx_i", stk)
        dsti = P.sb([128, NT, 4], I32, "dsti", stk)
        g12 = P.sb([128, NT, 2], F32, "g12", stk)
        rb = P.sb([128, NE], F32, "rb", stk)
        offs = P.sb([128, NE], F32, "offs", stk)
        tokid = P.sb([128, NT, 2], BF16, "tokid", stk)
        tokidf = P.sb([128, NT, 2], F32, "tokidf", stk)
        iota_f = P.sb([128, 128], F32, "iota_f", stk)
        ones_bf = P.sb([128, 128], BF16, "ones_bf", stk)
        ustr_f = P.sb([128, 128], F32, "ustr_f", stk)
        ustr_bf = P.sb([128, 128], BF16, "ustr_bf", stk)
        dma_load(cx, "sp", rb, rb[:], rb_bc, rb_bc[:])
        dma_load(cx, "sp", offs, offs[:], offs_d, offs_d[:])
        dma_load(cx, "sp", tokidf, tokidf[:], tokid_d, tokid_d[:])
        P.op("dve", _mk(nc.vector.tensor_copy, out=tokid[:], in_=tokidf[:]), reads=[tokidf], writes=[tokid])
        P.op("pool", _mk(nc.gpsimd.iota, iota_f[:], pattern=[[1, 128]], base=0, channel_multiplier=0, allow_small_or_imprecise_dtypes=True), writes=[iota_f])
        P.op("pool", _mk(nc.gpsimd.memset, ones_bf[:], 1.0), writes=[ones_bf])
        P.op("pool", _mk(nc.gpsimd.memset, ustr_f[:], 1.0), writes=[ustr_f])
        P.op("pool", _mk(nc.gpsimd.affine_select, out=ustr_f[:], in_=ustr_f[:], pattern=[[1, 128]], compare_op=ALU.is_ge, fill=0.0,
                         base=-1, channel_multiplier=-1), reads=[ustr_f], writes=[ustr_f])
        P.op("dve", _mk(nc.vector.tensor_copy, out=ustr_bf[:], in_=ustr_f[:]), reads=[ustr_f], writes=[ustr_bf])
        with contextlib.ExitStack() as stk2:
            wr = P.sb([128, KC, NE], F32, "wr", stk2)
            dma_load(cx, "sp", wr, wr[:], w_router_d, w_router_d[:].rearrange("(c p) n -> p c n", p=128))
            xt = [P.sb([128, D], F32, "rx", stk2) for _ in range(2)]
            xTf = [P.sb([128, KC, 128], F32, "rxT", stk2) for _ in range(2)]
            sm = [P.sb([128, NT, NE], F32, "rs%d" % j, stk2) for j in range(6)]
            sg = [P.sb([128, NT, 8], F32, "rg%d" % j, stk2) for j in range(4)]
            sd = P.sb([128, NT, 2], F32, "rden", stk2)
            sc, sel, eq, sel2, ge, gsel = sm
            m1, m2, gs, ohg = sg
            gm = P.sb([128, NT], F32, "rgm", stk2)
            for tt in range(NT):
                x, xT = xt[tt % 2], xTf[tt % 2]
                dma_load(cx, "sp", x, x[:], X1, X1[tt * 128:(tt + 1) * 128, :])
                for q in range(8):
                    bank = cx.banks[q % 4]
                    fns = [_mk(nc.tensor.transpose, bank[:, j * 128:(j + 1) * 128], x[:, (q * 4 + j) * 128:(q * 4 + j + 1) * 128], cx.identf[:])
                           for j in range(4)]
                    P.op("pe", fns, reads=[x, cx.identf], writes=[bank])
                    if q % 2 == 0:
                        P.op("act", _mk(nc.scalar.copy, out=xT[:, q * 4:(q + 1) * 4, :], in_=bank[:].rearrange("p (a b) -> p a b", a=4)), reads=[bank], writes=[xT])
                    else:
                        P.op("dve", _mk(nc.vector.tensor_copy, out=xT[:, q * 4:(q + 1) * 4, :], in_=bank[:].rearrange("p (a b) -> p a b", a=4)), reads=[bank], writes=[xT])
                lb = cx.banks[4 + tt % 2]
                fns = [_mk(nc.tensor.matmul, lb[:, 0:NE], xT[:, k, :], wr[:, k, :], start=(k == 0), stop=(k == KC - 1)) for k in range(KC)]
                P.op("pe", fns, reads=[xT, wr], writes=[lb])
                P.op("act", _mk(nc.scalar.activation, out=sc[:, tt, :], in_=lb[:, 0:NE], func=AF.Sigmoid), reads=[lb], writes=[sc])
            V = nc.vector
            g4 = lambda t: t[:].rearrange("p t (g k) -> p t g k", k=4)
            b4 = lambda t: t[:].unsqueeze(3).to_broadcast([128, NT, 8, 4])
            P.op("dve", _mk(V.tensor_tensor, out=sel[:], in0=sc[:], in1=rb[:].unsqueeze(1).to_broadcast([128, NT, NE]), op=ALU.add), reads=[sc, rb], writes=[sel])
            P.op("dve", _mk(V.tensor_reduce, out=m1[:], in_=g4(sel), axis=AX.X, op=ALU.max), reads=[sel], writes=[m1])
            P.op("dve", _mk(V.tensor_tensor, out=g4(eq), in0=g4(sel), in1=b4(m1), op=ALU.is_equal), reads=[sel, m1], writes=[eq])
            P.op("dve", _mk(V.scalar_tensor_tensor, out=sel2[:], in0=eq[:], scalar=-1e9, in1=sel[:], op0=ALU.mult, op1=ALU.add), reads=[eq, sel], writes=[sel2])
            P.op("dve", _mk(V.tensor_reduce, out=m2[:], in_=g4(sel2), axis=AX.X, op=ALU.max), reads=[sel2], writes=[m2])
            P.op("dve", _mk(V.tensor_tensor, out=gs[:], in0=m1[:], in1=m2[:], op=ALU.add), reads=[m1, m2], writes=[gs])
            P.op("dve", _mk(V.tensor_reduce, out=gm[:], in_=gs[:], axis=AX.X, op=ALU.max), reads=[gs], writes=[gm])
            P.op("dve", _mk(V.tensor_tensor, out=ohg[:], in0=gs[:], in1=gm[:].unsqueeze(2).to_broadcast([128, NT, 8]), op=ALU.is_equal), reads=[gs, gm], writes=[ohg])
            P.op("dve", _mk(V.tensor_tensor, out=g4(ge), in0=g4(sel), in1=b4(m2), op=ALU.is_ge), reads=[sel, m2], writes=[ge])
            P.op("dve", _mk(V.tensor_tensor, out=g4(A_all), in0=g4(ge), in1=b4(ohg), op=ALU.mult), reads=[ge, ohg], writes=[A_all])
            P.op("dve", _mk(V.tensor_copy, out=A_bf[:], in_=A_all[:]), reads=[A_all], writes=[A_bf])
            P.op("dve", _mk(V.tensor_tensor, out=gsel[:], in0=A_all[:], in1=sc[:], op=ALU.mult), reads=[A_all, sc], writes=[gsel])
            P.op("dve", _mk(V.tensor_reduce, out=sd[:, :, 0], in_=gsel[:], axis=AX.X, op=ALU.add), reads=[gsel], writes=[sd])
            P.op("dve", _mk(V.reciprocal, out=sd[:, :, 1], in_=sd[:, :, 0]), reads=[sd], writes=[sd])
            P.op("dve", _mk(V.tensor_tensor, out=gate_all[:], in0=gsel[:], in1=sd[:, :, 1:2].to_broadcast([128, NT, NE]), op=ALU.mult),
                 reads=[gsel, sd], writes=[gate_all])
            rbk = cx.banks[6]
            fns = []
            for tt in range(NT):
                for i in range(tt + 1):
                    fns.append(_mk(nc.tensor.matmul, rbk[:, tt * NE:(tt + 1) * NE], (ustr_bf if i == tt else ones_bf)[:], A_bf[:, i, :],
                                   start=(i == 0), stop=(i == tt)))
            P.op("pe", fns, reads=[A_bf, ustr_bf, ones_bf], writes=[rbk])
            P.op("act", _mk(nc.scalar.copy, out=rank_all[:].rearrange("p a b -> p (a b)"), in_=rbk[:, 0:NT * NE]), reads=[rbk], writes=[rank_all])
            vt = P.sb([128, NT, NE], F32, "vt", stk2)
            vm = P.sb([128, NT, NE], F32, "vm", stk2)
            fs = P.sb([128, NT, 4], F32, "fs", stk2)
            V = nc.vector
            P.op("dve", _mk(V.tensor_tensor, out=vt[:], in0=rank_all[:], in1=offs[:].unsqueeze(1).to_broadcast([128, NT, NE]), op=ALU.add),
                 reads=[rank_all, offs], writes=[vt])
            P.op("dve", _mk(V.tensor_tensor, out=vt[:], in0=vt[:], in1=A_all[:], op=ALU.mult), reads=[vt, A_all], writes=[vt])
            P.op("dve", _mk(V.tensor_reduce, out=fs[:, :, 0], in_=vt[:], axis=AX.X, op=ALU.max), reads=[vt], writes=[fs])
            P.op("dve", _mk(V.tensor_reduce, out=fs[:, :, 1], in_=vt[:], axis=AX.X, op=ALU.add), reads=[vt], writes=[fs])
            P.op("dve", _mk(V.tensor_tensor, out=fs[:, :, 1], in0=fs[:, :, 1], in1=fs[:, :, 0], op=ALU.subtract), reads=[fs], writes=[fs])
            P.op("dve", _mk(V.tensor_tensor, out=vm[:], in0=vt[:], in1=fs[:, :, 0:1].to_broadcast([128, NT, NE]), op=ALU.is_equal),
                 reads=[vt, fs], writes=[vm])
            P.op("dve", _mk(V.tensor_tensor, out=vm[:], in0=vm[:], in1=gate_all[:], op=ALU.mult), reads=[vm, gate_all], writes=[vm])
            P.op("dve", _mk(V.tensor_reduce, out=g12[:, :, 0], in_=vm[:], axis=AX.X, op=ALU.add), reads=[vm], writes=[g12])
            P.op("dve", _mk(V.tensor_reduce, out=fs[:, :, 2], in_=gate_all[:], axis=AX.X, op=ALU.add), reads=[gate_all], writes=[fs])
            P.op("dve", _mk(V.tensor_tensor, out=g12[:, :, 1], in0=fs[:, :, 2], in1=g12[:, :, 0], op=ALU.subtract), reads=[fs, g12], writes=[g12])
            P.op("dve", _mk(V.tensor_scalar, out=fs[:, :, 0:2], in0=fs[:, :, 0:2], scalar1=-1.0, scalar2=float(NE * CAP - 1), op0=ALU.add, op1=ALU.min),
                 reads=[fs], writes=[fs])
            P.op("dve", _mk(V.tensor_scalar_max, out=fs[:, :, 0:2], in0=fs[:, :, 0:2], scalar1=0.0), reads=[fs], writes=[fs])
            fs2 = P.sb([128, NT, 4], F32, "fs2", stk2)
            for k in range(2):
                P.op("dve", _mk(V.tensor_scalar, out=fs2[:, :, 2 * k:2 * k + 1], in0=fs[:, :, k:k + 1], scalar1=2.0, scalar2=None, op0=ALU.mult), reads=[fs], writes=[fs2])
                P.op("dve", _mk(V.tensor_scalar, out=fs2[:, :, 2 * k + 1:2 * k + 2], in0=fs[:, :, k:k + 1], scalar1=2.0, scalar2=1.0, op0=ALU.mult, op1=ALU.add),
                     reads=[fs], writes=[fs2])
            P.op("dve", _mk(V.tensor_copy, out=dsti[:], in_=fs2[:]), reads=[fs2], writes=[dsti])
            pe_t = [P.sb([128, NT, 128], BF16, "pe_t", stk2) for _ in range(2)]
            ibk = cx.banks[7]
            for e in range(NE):
                pt = pe_t[e % 2]
                for tt in range(NT):
                    P.op("dve", _mk(V.tensor_scalar, out=pt[:, tt, :], in0=iota_f[:], scalar1=rank_all[:, tt, e:e + 1], scalar2=A_all[:, tt, e:e + 1],
                                    op0=ALU.is_equal, op1=ALU.mult), reads=[iota_f, rank_all, A_all], writes=[pt])
                fns = [_mk(nc.tensor.matmul, ibk[:, e * 2:(e + 1) * 2], pt[:, tt, :], tokid[:, tt, :], start=(tt == 0), stop=(tt == NT - 1)) for tt in range(NT)]
                P.op("pe", fns, reads=[pt, tokid], writes=[ibk])
            idxf = P.sb([128, NE, 2], F32, "idxf", stk2)
            idx1 = P.sb([128, NE], F32, "idx1", stk2)
            P.op("act", _mk(nc.scalar.copy, out=idxf[:].rearrange("p a b -> p (a b)"), in_=ibk[:, 0:2 * NE]), reads=[ibk], writes=[idxf])
            P.op("dve", _mk(V.scalar_tensor_tensor, out=idx1[:], in0=idxf[:, :, 1], scalar=128.0, in1=idxf[:, :, 0], op0=ALU.mult, op1=ALU.add),
                 reads=[idxf], writes=[idx1])
            P.op("dve", _mk(V.tensor_copy, out=idx_i[:], in_=idx1[:]), reads=[idx1], writes=[idx_i])
            if dbg is not None:
                for nm, t in (("A_all", A_all), ("gate_all", gate_all), ("rank_all", rank_all), ("idx_i", idx_i), ("dsti", dsti), ("g12", g12)):
                    dma_store(cx, "sp", dbg[nm], dbg[nm][:], t, t[:])
            P.barrier()
            P.emit()
        if stop_after == "router":
            return
        with contextlib.ExitStack() as stk2:
            xg = [P.sb([128, D], BF16, "xg", stk2) for _ in range(2)]
            xgT = [P.sb([128, KC, 128], BF16, "xgT", stk2) for _ in range(2)]
            wt_s = [P.sb([128, KC, 256], BF16, "wie", stk2) for _ in range(3)]
            wo_s = [P.sb([128, 6, 1024], BF16, "woe", stk2) for _ in range(3)]
            hs = [P.sb([128, 2 * DFF], F32, "hs", stk2) for _ in range(2)]
            act_b = [P.sb([128, DFF], BF16, "actb", stk2) for _ in range(2)]
            actT = [P.sb([128, 6, 128], BF16, "actT", stk2) for _ in range(2)]
            ybs = [P.sb([128, D], F32, "ybs", stk2) for _ in range(2)]
            wi_n = 0
            wo_n = 0
            for e in range(NE):
                g, gT, h, ab, aT, yb = xg[e % 2], xgT[e % 2], hs[e % 2], act_b[e % 2], actT[e % 2], ybs[e % 2]
                P.dma("pool", _mk(nc.gpsimd.indirect_dma_start, out=g[:], out_offset=None, in_=X1b[:, :],
                                  in_offset=bass.IndirectOffsetOnAxis(ap=idx_i[:, e:e + 1], axis=0)),
                      g, reads=[X1b, idx_i], writes=[g])
                for q in range(4):
                    bank = cx.banks[q]
                    bv = bank[:].bitcast(BF16).rearrange("p (a b) -> p a b", a=8)
                    fns = [_mk(nc.tensor.transpose, bv[:, j, :], g[:, (q * 8 + j) * 128:(q * 8 + j + 1) * 128], cx.identb[:]) for j in range(8)]
                    P.op("pe", fns, reads=[g, cx.identb], writes=[bank])
                    if q % 2 == 0:
                        P.op("act", _mk(nc.scalar.copy, out=gT[:, q * 8:(q + 1) * 8, :], in_=bv), reads=[bank], writes=[gT])
                    else:
                        P.op("dve", _mk(nc.vector.tensor_copy, out=gT[:, q * 8:(q + 1) * 8, :], in_=bv), reads=[bank], writes=[gT])
                for c in range(6):
                    wt = wt_s[wi_n % 3]
                    wi_n += 1
                    load_w_cols(cx, wt, w_in_e, c * 256, 256, KC, lead=(e,))
                    bank = cx.banks[4 + c % 2]
                    fns = [_mk(nc.tensor.matmul, bank[:, 0:256], gT[:, k, :], wt[:, k, :], start=(k == 0), stop=(k == KC - 1)) for k in range(KC)]
                    P.op("pe", fns, reads=[gT, wt], writes=[bank])
                    if c < 3:
                        P.op("act", _mk(nc.scalar.activation, out=h[:, c * 256:(c + 1) * 256], in_=bank[:, 0:256], func=AF.Silu), reads=[bank], writes=[h])
                    else:
                        P.op("act", _mk(nc.scalar.copy, out=h[:, c * 256:(c + 1) * 256], in_=bank[:, 0:256]), reads=[bank], writes=[h])
                P.op("dve", _mk(nc.vector.tensor_tensor, out=ab[:], in0=h[:, 0:DFF], in1=h[:, DFF:2 * DFF], op=ALU.mult), reads=[h], writes=[ab])
                bank = cx.banks[6]
                bv = bank[:].bitcast(BF16).rearrange("p (a b) -> p a b", a=8)
                fns = [_mk(nc.tensor.transpose, bv[:, j, :], ab[:, j * 128:(j + 1) * 128], cx.identb[:]) for j in range(6)]
                P.op("pe", fns, reads=[ab, cx.identb], writes=[bank])
                P.op("dve", _mk(nc.vector.tensor_copy, out=aT[:], in_=bv[:, 0:6, :]), reads=[bank], writes=[aT])
                for c4 in range(4):
                    wo = wo_s[wo_n % 3]
                    wo_n += 1
                    load_w_cols(cx, wo, w_out_e, c4 * 1024, 1024, 6, lead=(e,))
                    for c2 in range(2):
                        c = c4 * 2 + c2
                        bank = cx.banks[c % 4]
                        fns = [_mk(nc.tensor.matmul, bank[:, :], aT[:, k, :], wo[:, k, c2 * 512:(c2 + 1) * 512], start=(k == 0), stop=(k == 5)) for k in range(6)]
                        P.op("pe", fns, reads=[aT, wo], writes=[bank])
                        if c % 2 == 0:
                            P.op("act", _mk(nc.scalar.copy, out=yb[:, c * 512:(c + 1) * 512], in_=bank[:, :]), reads=[bank], writes=[yb])
                        else:
                            P.op("dve", _mk(nc.vector.tensor_copy, out=yb[:, c * 512:(c + 1) * 512], in_=bank[:, :]), reads=[bank], writes=[yb])
                dma_store(cx, "sp", YB, YB[e * CAP:(e + 1) * CAP, :], yb, yb[:])
            P.barrier()
            P.emit()
        if stop_after == "experts":
            return
        with contextlib.ExitStack() as stk2:
            r1 = [P.sb([128, D], F32, "r1", stk2) for _ in range(2)]
            r2 = [P.sb([128, D], F32, "r2", stk2) for _ in range(2)]
            xr = [P.sb([128, D], F32, "xr", stk2) for _ in range(2)]
            for tt in range(NT):
                a, b, x = r1[tt % 2], r2[tt % 2], xr[tt % 2]
                for k, r in ((0, a), (1, b)):
                    for hf in range(2):
                        P.dma("pool", _mk(nc.gpsimd.indirect_dma_start, out=r[:, hf * 2048:(hf + 1) * 2048], out_offset=None,
                                          in_=YB[:, :].rearrange("s (h c) -> (s h) c", h=2),
                                          in_offset=bass.IndirectOffsetOnAxis(ap=dsti[:, tt, 2 * k + hf:2 * k + hf + 1], axis=0)),
                              r, reads=[YB, dsti], writes=[r])
                dma_load(cx, "sp", x, x[:], X1, X1[tt * 128:(tt + 1) * 128, :])
                V = nc.vector
                P.op("act", _mk(nc.scalar.mul, out=x[:], in_=x[:], mul=ALPHA), reads=[x], writes=[x])
                P.op("dve", _mk(V.scalar_tensor_tensor, out=x[:], in0=a[:], scalar=g12[:, tt, 0:1], in1=x[:], op0=ALU.mult, op1=ALU.add),
                     reads=[a, g12, x], writes=[x])
                P.op("dve", _mk(V.scalar_tensor_tensor, out=x[:], in0=b[:], scalar=g12[:, tt, 1:2], in1=x[:], op0=ALU.mult, op1=ALU.add),
                     reads=[b, g12, x], writes=[x])
                dma_store(cx, "sp", Y, Y[tt * 128:(tt + 1) * 128, :], x, x[:])
            P.barrier()
            P.emit()


DI = 8192
NH = 128
NG = 8
HPG = 16
HD = 64
NS = 128
ZOFF, XOFF, BOFF, COFF, DTOFF = 0, DI, 2 * DI, 2 * DI + NG * NS, 2 * DI + 2 * NG * NS
RMS_EPS = 1e-5


def ssd_consts(cx, stk):
    nc, P = cx.nc, cx.P
    c = {}
    c["uincl"] = P.sb([128, 128], F32, "uincl", stk)
    c["onesf"] = P.sb([128, 128], F32, "onesf", stk)
    c["mask"] = P.sb([128, 128], F32, "maskf", stk)
    P.op("pool", _mk(nc.gpsimd.memset, c["onesf"][:], 1.0), writes=[c["onesf"]])
    P.op("pool", _mk(nc.gpsimd.memset, c["uincl"][:], 1.0), writes=[c["uincl"]])
    P.op("pool", _mk(nc.gpsimd.affine_select, out=c["uincl"][:], in_=c["uincl"][:], pattern=[[1, 128]], compare_op=ALU.is_ge, fill=0.0,
                     base=0, channel_multiplier=-1), reads=[c["uincl"]], writes=[c["uincl"]])
    P.op("pool", _mk(nc.gpsimd.memset, c["mask"][:], 0.0), writes=[c["mask"]])
    P.op("pool", _mk(nc.gpsimd.affine_select, out=c["mask"][:], in_=c["mask"][:], pattern=[[1, 128]], compare_op=ALU.is_ge, fill=-1e30,
                     base=0, channel_multiplier=-1), reads=[c["mask"]], writes=[c["mask"]])
    return c


def stage_ssd_a(cx, x2h, w_in, convw_fm, convb_fm, dtb_bc, alog_bc, dsk_bc, YL, SL, TOTC, EAG, CTd, ACTd):
    nc, P = cx.nc, cx.P
    V, G, S = nc.vector, nc.gpsimd, nc.scalar
    TH = T + HALO
    with contextlib.ExitStack() as stk:
        x2T = P.sb([128, KC, TH], BF16, "x2T", stk)
        with contextlib.ExitStack() as stk3:
            fm_convert(cx, stk3, x2h, 0, TH, x2T, 0)
            P.barrier()
            P.emit()
        K = ssd_consts(cx, stk)
        wsl = [P.sb([128, KC, 128], BF16, "wsl", stk) for _ in range(2)]
        cw = P.sb([128, 80, 4], F32, "cw", stk)
        cb = P.sb([128, 80], F32, "cb", stk)
        dtb = P.sb([128, NH], F32, "dtb", stk)
        aneg = P.sb([128, NH], F32, "aneg", stk)
        dsk = P.sb([128, NH], F32, "dsk", stk)
        dma_load(cx, "sp", cw, cw[:], convw_fm, convw_fm[:])
        dma_load(cx, "sp", cb, cb[:], convb_fm, convb_fm[:])
        dma_load(cx, "sp", dtb, dtb[:], dtb_bc, dtb_bc[:])
        dma_load(cx, "sp", aneg, aneg[:], alog_bc, alog_bc[:])
        dma_load(cx, "sp", dsk, dsk[:], dsk_bc, dsk_bc[:])
        P.op("act", _mk(S.activation, out=aneg[:], in_=aneg[:], func=AF.Exp), reads=[aneg], writes=[aneg])
        P.op("dve", _mk(V.tensor_scalar, out=aneg[:], in0=aneg[:], scalar1=-1.0, scalar2=None, op0=ALU.mult), reads=[aneg], writes=[aneg])
        dt = P.sb([128, NT, NH], F32, "dt", stk)
        dtA = P.sb([128, NT, NH], F32, "dtA", stk)
        acum = P.sb([128, NT, NH], F32, "acum", stk)
        eac = P.sb([128, NT, NH], F32, "eac", stk)
        dend = P.sb([128, NT, NH], F32, "dend", stk)
        cdec = P.sb([128, NT, NH], F32, "cdec", stk)
        eag = P.sb([128, NT, NH], F32, "eag", stk)
        run = P.sb([128, NH], F32, "run", stk)
        tmp = [P.sb([128, NH], F32, "dtt%d" % i, stk) for i in range(4)]
        wt = wsl[0]
        load_w_cols(cx, wt, w_in, DTOFF, NH, KC)
        P.op("pool", _mk(G.memset, run[:], 0.0), writes=[run])
        for c in range(NT):
            bank = cx.banks[c % 2]
            fns = [_mk(nc.tensor.matmul, bank[:, 0:NH], x2T[:, k, HALO + c * 128:HALO + (c + 1) * 128], wt[:, k, :], start=(k == 0), stop=(k == KC - 1))
                   for k in range(KC)]
            P.op("pe", fns, reads=[x2T, wt], writes=[bank])
            xr, ax, ex, lx = tmp
            P.op("dve", _mk(V.tensor_tensor, out=xr[:], in0=bank[:, 0:NH], in1=dtb[:], op=ALU.add), reads=[bank, dtb], writes=[xr])
            P.op("act", _mk(S.activation, out=ax[:], in_=xr[:], func=AF.Abs), reads=[xr], writes=[ax])
            P.op("act", _mk(S.activation, out=ex[:], in_=ax[:], func=AF.Exp, scale=-1.0), reads=[ax], writes=[ex])
            P.op("act", _mk(S.activation, out=lx[:], in_=ex[:], func=AF.Ln, bias=1.0, scale=1.0), reads=[ex], writes=[lx])
            P.op("dve", _mk(V.scalar_tensor_tensor, out=dt[:, c, :], in0=xr[:], scalar=0.0, in1=lx[:], op0=ALU.max, op1=ALU.add), reads=[xr, lx], writes=[dt])
            P.op("dve", _mk(V.tensor_tensor, out=dtA[:, c, :], in0=dt[:, c, :], in1=aneg[:], op=ALU.mult), reads=[dt, aneg], writes=[dtA])
            b2 = cx.banks[2 + c % 2]
            P.op("pe", [_mk(nc.tensor.matmul, b2[:, 0:NH], K["uincl"][:], dtA[:, c, :], start=True, stop=True),
                        _mk(nc.tensor.matmul, b2[:, NH:2 * NH], K["onesf"][:], dtA[:, c, :], start=True, stop=True)],
                 reads=[K["uincl"], K["onesf"], dtA], writes=[b2])
            P.op("act", _mk(S.copy, out=acum[:, c, :], in_=b2[:, 0:NH]), reads=[b2], writes=[acum])
            P.op("act", _mk(S.activation, out=eac[:, c, :], in_=b2[:, 0:NH], func=AF.Exp), reads=[b2], writes=[eac])
            P.op("act", _mk(S.activation, out=cdec[:, c, :], in_=b2[:, NH:2 * NH], func=AF.Exp), reads=[b2], writes=[cdec])
            P.op("dve", _mk(V.tensor_tensor, out=ax[:], in0=b2[:, NH:2 * NH], in1=acum[:, c, :], op=ALU.subtract), reads=[b2, acum], writes=[ax])
            P.op("act", _mk(S.activation, out=dend[:, c, :], in_=ax[:], func=AF.Exp), reads=[ax], writes=[dend])
            P.op("dve", _mk(V.tensor_tensor, out=ex[:], in0=acum[:, c, :], in1=run[:], op=ALU.add), reads=[acum, run], writes=[ex])
            P.op("act", _mk(S.activation, out=eag[:, c, :], in_=ex[:], func=AF.Exp), reads=[ex], writes=[eag])
            P.op("dve", _mk(V.tensor_tensor, out=run[:], in0=run[:], in1=b2[:, NH:2 * NH], op=ALU.add), reads=[run, b2], writes=[run])
            b3 = cx.banks[4 + c % 2]
            P.op("pe", _mk(nc.tensor.transpose, b3[:, 0:128], acum[:, c, :], cx.identf[:]), reads=[acum, cx.identf], writes=[b3])
            P.op("dve", _mk(V.tensor_copy, out=lx[:], in_=b3[:, 0:128]), reads=[b3], writes=[lx])
            dma_store(cx, "sp", ACTd, ACTd[:, c * 128:(c + 1) * 128], lx, lx[:])
        dma_store(cx, "sp", TOTC, TOTC[:], run, run[:])
        dma_store(cx, "sp", EAG, EAG[:], eag, eag[:])
        P.barrier()
        P.emit()
        ub = [P.sb([128, TH], F32, "ub", stk) for _ in range(2)]
        acc = [P.sb([128, T], F32, "cacc", stk) for _ in range(2)]
        fT = [P.sb([128, T], BF16, "fT", stk) for _ in range(2)]
        xs_tok = P.sb([128, NT, 1024], BF16, "xs_tok", stk)
        b_tok = P.sb([128, NT, NS], BF16, "b_tok", stk)
        BT = P.sb([128, T], BF16, "BT", stk)
        CT = P.sb([128, T], BF16, "CT", stk)
        state = P.sb([128, 1024], F32, "state", stk)
        state_bf = P.sb([128, 1024], BF16, "state_bf", stk)
        acb = [P.sb([128, 8, 128], F32, "acb", stk) for _ in range(2)]
        Et = [P.sb([128, 8, 128], F32, "Et", stk) for _ in range(2)]
        Mt = [P.sb([128, 8, 128], BF16, "Mt", stk) for _ in range(2)]
        cbT = [P.sb([128, 128], F32, "cbT", stk) for _ in range(2)]
        xs_dt = [P.sb([128, 1024], BF16, "xs_dt", stk) for _ in range(2)]
        xs_dd = [P.sb([128, 1024], BF16, "xs_dd", stk) for _ in range(2)]
        yt = [P.sb([128, 1024], F32, "yt", stk) for _ in range(2)]
        y2 = [P.sb([128, 1024], F32, "y2", stk)] * 2
        segs = [(0, 512), (512, 512), (1024, TH - 1024)]
        nload = [0]

        def proj_chunk(col, j, out_fT):
            wt = wsl[nload[0] % 2]
            u, a = ub[nload[0] % 2], acc[nload[0] % 2]
            nload[0] += 1
            load_w_cols(cx, wt, w_in, col, 128, KC)
            for si, (t0, n) in enumerate(segs):
                bank = cx.banks[si]
                fns = [_mk(nc.tensor.matmul, bank[:, 0:n], wt[:, k, :], x2T[:, k, t0:t0 + n], start=(k == 0), stop=(k == KC - 1)) for k in range(KC)]
                P.op("pe", fns, reads=[wt, x2T], writes=[bank])
                P.op("act", _mk(S.copy, out=u[:, t0:t0 + n], in_=bank[:, 0:n]), reads=[bank], writes=[u])
            P.op("dve", _mk(V.tensor_scalar, out=a[:], in0=u[:, HALO - 3:HALO - 3 + T], scalar1=cw[:, j, 0:1], scalar2=None, op0=ALU.mult), reads=[u, cw], writes=[a])
            for k in range(1, 4):
                P.op("dve", _mk(V.scalar_tensor_tensor, out=a[:], in0=u[:, HALO - 3 + k:HALO - 3 + k + T], scalar=cw[:, j, k:k + 1], in1=a[:],
                                op0=ALU.mult, op1=ALU.add), reads=[u, cw, a], writes=[a])
            P.op("act", _mk(S.activation, out=out_fT[:], in_=a[:], func=AF.Silu, bias=cb[:, j:j + 1], scale=1.0), reads=[a, cb], writes=[out_fT])

        for g in range(NG):
            for j8 in range(8):
                f = fT[j8 % 2]
                proj_chunk(XOFF + g * 1024 + j8 * 128, g * 8 + j8, f)
                bank = cx.banks[3 + j8 % 2]
                bv = bank[:].bitcast(BF16).rearrange("p (a b) -> p a b", a=8)
                fns = [_mk(nc.tensor.transpose, bv[:, c, :], f[:, c * 128:(c + 1) * 128], cx.identb[:]) for c in range(NT)]
                P.op("pe", fns, reads=[f, cx.identb], writes=[bank])
                P.op("dve" if j8 % 2 else "act",
                     _mk(V.tensor_copy, out=xs_tok[:, :, j8 * 128:(j8 + 1) * 128], in_=bv) if j8 % 2 else
                     _mk(S.copy, out=xs_tok[:, :, j8 * 128:(j8 + 1) * 128], in_=bv), reads=[bank], writes=[xs_tok])
            proj_chunk(BOFF + g * NS, 64 + g, BT)
            bank = cx.banks[3]
            bv = bank[:].bitcast(BF16).rearrange("p (a b) -> p a b", a=8)
            fns = [_mk(nc.tensor.transpose, bv[:, c, :], BT[:, c * 128:(c + 1) * 128], cx.identb[:]) for c in range(NT)]
            P.op("pe", fns, reads=[BT, cx.identb], writes=[bank])
            P.op("act", _mk(S.copy, out=b_tok[:], in_=bv), reads=[bank], writes=[b_tok])
            proj_chunk(COFF + g * NS, 72 + g, CT)
            dma_store(cx, "sp", CTd, CTd[g, :, :], CT, CT[:])
            P.op("pool", _mk(G.memset, state[:], 0.0), writes=[state])
            P.op("pool", _mk(G.memset, state_bf[:], 0.0), writes=[state_bf])
            hs = slice(g * HPG, (g + 1) * HPG)
            for c in range(NT):
                i = g * NT + c
                ts = slice(c * 128, (c + 1) * 128)
                xd, xdd, y, yb2, cb_s = xs_dt[i % 2], xs_dd[i % 2], yt[i % 2], y2[i % 2], cbT[i % 2]
                v3 = lambda t: t[:].rearrange("p (h d) -> p h d", d=HD)
                bc_hd = lambda ap: ap.unsqueeze(2).to_broadcast([128, HPG, HD])
                P.op("dve", _mk(V.tensor_tensor, out=v3(xd), in0=xs_tok[:, c, :].rearrange("p (h d) -> p h d", d=HD), in1=bc_hd(dt[:, c, hs]), op=ALU.mult),
                     reads=[xs_tok, dt], writes=[xd])
                P.op("pool", _mk(G.tensor_tensor, out=v3(xdd), in0=v3(xd), in1=bc_hd(dend[:, c, hs]), op=ALU.mult), reads=[xd, dend], writes=[xdd])
                bk0 = cx.banks[0]
                P.op("pe", _mk(nc.tensor.matmul, bk0[:, 0:128], BT[:, ts], CT[:, ts], start=True, stop=True), reads=[BT, CT], writes=[bk0])
                P.op("act", _mk(S.copy, out=cb_s[:], in_=bk0[:, 0:128]), reads=[bk0], writes=[cb_s])
                if c > 0:
                    for hf in range(2):
                        bk = cx.banks[3 + hf]
                        P.op("pe", _mk(nc.tensor.matmul, bk[:, :], CT[:, ts], state_bf[:, hf * 512:(hf + 1) * 512], start=True, stop=True),
                             reads=[CT, state_bf], writes=[bk])
                for hf in range(2):
                    ab, E, M = acb[(2 * i + hf) % 2], Et[(2 * i + hf) % 2], Mt[(2 * i + hf) % 2]
                    h0 = g * HPG + hf * 8
                    P.dma("sp", _mk(nc.sync.dma_start, out=ab[:], in_=ACTd[h0:h0 + 8, ts].partition_broadcast(128)), ab, reads=[ACTd], writes=[ab])
                    P.op("pool", _mk(G.tensor_tensor, out=ab[:], in0=ab[:], in1=K["mask"][:].unsqueeze(1).to_broadcast([128, 8, 128]), op=ALU.add),
                         reads=[ab, K["mask"]], writes=[ab])
                    P.op("dve", _mk(V.tensor_tensor, out=E[:], in0=ab[:], in1=acum[:, c, h0:h0 + 8].unsqueeze(2).to_broadcast([128, 8, 128]), op=ALU.subtract),
                         reads=[ab, acum], writes=[E])
                    P.op("act", _mk(S.activation, out=E[:], in_=E[:], func=AF.Exp), reads=[E], writes=[E])
                    P.op("dve", _mk(V.tensor_tensor, out=M[:], in0=E[:], in1=cb_s[:].unsqueeze(1).to_broadcast([128, 8, 128]), op=ALU.mult),
                         reads=[E, cb_s], writes=[M])
                    bk = cx.banks[1 + hf]
                    fns = [_mk(nc.tensor.matmul, bk[:, hh * HD:(hh + 1) * HD], M[:, hh, :], xd[:, (hf * 8 + hh) * HD:(hf * 8 + hh + 1) * HD], start=True, stop=True)
                           for hh in range(8)]
                    P.op("pe", fns, reads=[M, xd], writes=[bk])
                for hf in range(2):
                    bk = cx.banks[5 + hf]
                    P.op("pe", _mk(nc.tensor.matmul, bk[:, :], b_tok[:, c, :], xdd[:, hf * 512:(hf + 1) * 512], start=True, stop=True), reads=[b_tok, xdd], writes=[bk])
                for hf in range(2):
                    ys = slice(hf * 512, (hf + 1) * 512)
                    hh = slice(g * HPG + hf * 8, g * HPG + hf * 8 + 8)
                    v8 = lambda ap: ap.rearrange("p (h d) -> p h d", d=HD)
                    b8 = lambda ap: ap.unsqueeze(2).to_broadcast([128, 8, HD])
                    P.op("dve", _mk(V.tensor_tensor, out=v8(y[:, ys]), in0=v8(xs_tok[:, c, ys]), in1=b8(dsk[:, hh]), op=ALU.mult), reads=[xs_tok, dsk], writes=[y])
                    P.op("dve", _mk(V.tensor_tensor, out=y[:, ys], in0=y[:, ys], in1=cx.banks[1 + hf][:, :], op=ALU.add), reads=[y, cx.banks[1 + hf]], writes=[y])
                    if c > 0:
                        P.op("dve", _mk(V.tensor_tensor, out=v8(yb2[:, ys]), in0=v8(cx.banks[3 + hf][:, :]), in1=b8(eac[:, c, hh]), op=ALU.mult),
                             reads=[cx.banks[3 + hf], eac], writes=[yb2])
                        P.op("pool", _mk(G.tensor_tensor, out=y[:, ys], in0=y[:, ys], in1=yb2[:, ys], op=ALU.add), reads=[y, yb2], writes=[y])
                dma_store(cx, "sp", YL, YL[ts, g * 1024:(g + 1) * 1024], y, y[:])
                for hf in range(2):
                    ys = slice(hf * 512, (hf + 1) * 512)
                    hh = slice(g * HPG + hf * 8, g * HPG + hf * 8 + 8)
                    v8 = lambda ap: ap.rearrange("p (h d) -> p h d", d=HD)
                    b8 = lambda ap: ap.unsqueeze(2).to_broadcast([128, 8, HD])
                    P.op("dve", _mk(V.tensor_tensor, out=v8(state[:, ys]), in0=v8(state[:, ys]), in1=b8(cdec[:, c, hh]), op=ALU.mult), reads=[state, cdec], writes=[state])
                    P.op("dve", _mk(V.tensor_tensor, out=state[:, ys], in0=state[:, ys], in1=cx.banks[5 + hf][:, :], op=ALU.add), reads=[state, cx.banks[5 + hf]], writes=[state])
                P.op("act", _mk(S.copy, out=state_bf[:], in_=state[:]), reads=[state], writes=[state_bf])
            dma_store(cx, "sp", SL, SL[:, g * 1024:(g + 1) * 1024], state, state[:])
        P.barrier()
        P.emit()


def stage_ssd_b(cx, x2h, w_in, w_out, YL, EAG, CTd, S_all, TOT_all, Wk_d, negm_d, normw_bc, YN, Y):
    nc, P = cx.nc, cx.P
    V, G, S = nc.vector, nc.gpsimd, nc.scalar
    TH = T + HALO
    with contextlib.ExitStack() as stk:
        hin_bf = P.sb([128, DI], BF16, "hin_bf", stk)
        eag = P.sb([128, NT, NH], F32, "eagb", stk)
        dma_load(cx, "sp", eag, eag[:], EAG, EAG[:])
        with contextlib.ExitStack() as stk2:
            wk = P.sb([NCORES, NCORES, 128], F32, "wk", stk2)
            tot = P.sb([NCORES, NH], F32, "tot", stk2)
            negm = P.sb([128, NCORES], F32, "negm", stk2)
            coef = P.sb([128, NCORES, NH], F32, "coef", stk2)
            hacc = P.sb([128, DI], F32, "hacc", stk2)
            sj = [P.sb([128, DI], F32, "sj", stk2) for _ in range(2)]
            dma_load(cx, "sp", wk, wk[:], Wk_d, Wk_d[:])
            dma_load(cx, "sp", tot, tot[:], TOT_all, TOT_all[:])
            dma_load(cx, "sp", negm, negm[:], negm_d, negm_d[:])
            for j in range(NCORES):
                bank = cx.banks[j % 2]
                P.op("pe", _mk(nc.tensor.matmul, bank[:, 0:NH], wk[:, j, :], tot[:], start=True, stop=True), reads=[wk, tot], writes=[bank])
                P.op("act", _mk(S.activation, out=coef[:, j, :], in_=bank[:, 0:NH], func=AF.Exp, bias=negm[:, j:j + 1], scale=1.0), reads=[bank, negm], writes=[coef])
                s = sj[j % 2]
                dma_load(cx, "sp", s, s[:], S_all, S_all[j])
                v3 = lambda ap: ap.rearrange("p (h d) -> p h d", d=HD)
                cbc = coef[:, j, :].unsqueeze(2).to_broadcast([128, NH, HD])
                if j == 0:
                    P.op("dve", _mk(V.tensor_tensor, out=v3(hacc[:]), in0=v3(s[:]), in1=cbc, op=ALU.mult), reads=[s, coef], writes=[hacc])
                else:
                    P.op("pool", _mk(G.tensor_tensor, out=v3(s[:]), in0=v3(s[:]), in1=cbc, op=ALU.mult), reads=[s, coef], writes=[s])
                    P.op("dve", _mk(V.tensor_tensor, out=hacc[:], in0=hacc[:], in1=s[:], op=ALU.add), reads=[hacc, s], writes=[hacc])
            P.op("act", _mk(S.copy, out=hin_bf[:], in_=hacc[:]), reads=[hacc], writes=[hin_bf])
            P.barrier()
            P.emit()
        with contextlib.ExitStack() as stk2:
            x2T = P.sb([128, KC, TH], BF16, "x2Tb", stk2)
            with contextlib.ExitStack() as stk3:
                fm_convert(cx, stk3, x2h, 0, TH, x2T, 0)
                P.barrier()
                P.emit()
            y_g0 = P.sb([128, NT, 1024], F32, "y_g", stk2)
            y_gt = [Tile(P, y_g0.h, "y_g%d" % i) for i in range(NT)]
            CT = [P.sb([128, T], BF16, "CTb", stk2) for _ in range(2)]
            nw = [P.sb([128, 1024], F32, "nwb", stk2) for _ in range(2)]
            wz = [P.sb([128, KC, 256], BF16, "wz", stk2) for _ in range(3)]
            tmpc = [P.sb([128, 512], F32, "tmpc", stk2) for _ in range(2)]
            sz = [P.sb([128, 256], F32, "sz", stk2) for _ in range(2)]
            sq = P.sb([128, 1024], F32, "sq", stk2)
            ss = [P.sb([128, 4], F32, "ss", stk2) for _ in range(2)]
            ynb = [P.sb([128, 1024], BF16, "ynb", stk2) for _ in range(2)]
            nz = 0
            for g in range(NG):
                ct, nwg = CT[g % 2], nw[g % 2]
                dma_load(cx, "sp", ct, ct[:], CTd, CTd[g, :, :])
                dma_load(cx, "sp", nwg, nwg[:], normw_bc, normw_bc[:, g * 1024:(g + 1) * 1024])
                for tt in range(NT):
                    ts = slice(tt * 128, (tt + 1) * 128)
                    y_g = y_gt[tt]
                    dma_load(cx, "sp", y_g, y_g[:, tt, :], YL, YL[ts, g * 1024:(g + 1) * 1024])
                    for hf in range(2):
                        bank = cx.banks[(tt * 2 + hf) % 4]
                        tc_ = tmpc[(tt * 2 + hf) % 2]
                        hh = slice(g * HPG + hf * 8, g * HPG + hf * 8 + 8)
                        P.op("pe", _mk(nc.tensor.matmul, bank[:, :], ct[:, ts], hin_bf[:, g * 1024 + hf * 512:g * 1024 + (hf + 1) * 512], start=True, stop=True),
                             reads=[ct, hin_bf], writes=[bank])
                        P.op("dve", _mk(V.tensor_tensor, out=tc_[:].rearrange("p (h d) -> p h d", d=HD), in0=bank[:, :].rearrange("p (h d) -> p h d", d=HD),
                                        in1=eag[:, tt, hh].unsqueeze(2).to_broadcast([128, 8, HD]), op=ALU.mult), reads=[bank, eag], writes=[tc_])
                        P.op("pool", _mk(G.tensor_tensor, out=y_g[:, tt, hf * 512:(hf + 1) * 512], in0=y_g[:, tt, hf * 512:(hf + 1) * 512], in1=tc_[:], op=ALU.add),
                             reads=[y_g, tc_], writes=[y_g])
                for zc in range(4):
                    w = wz[nz % 3]
                    nz += 1
                    load_w_cols(cx, w, w_in, ZOFF + g * 1024 + zc * 256, 256, KC)
                    for tt in range(NT):
                        y_g = y_gt[tt]
                        bank = cx.banks[4 + tt % 4]
                        s_ = sz[tt % 2]
                        fns = [_mk(nc.tensor.matmul, bank[:, 0:256], x2T[:, k, HALO + tt * 128:HALO + (tt + 1) * 128], w[:, k, :], start=(k == 0), stop=(k == KC - 1))
                               for k in range(KC)]
                        P.op("pe", fns, reads=[x2T, w], writes=[bank])
                        P.op("act", _mk(S.activation, out=s_[:], in_=bank[:, 0:256], func=AF.Silu), reads=[bank], writes=[s_])
                        P.op("dve", _mk(V.tensor_tensor, out=y_g[:, tt, zc * 256:(zc + 1) * 256], in0=y_g[:, tt, zc * 256:(zc + 1) * 256], in1=s_[:], op=ALU.mult),
                             reads=[y_g, s_], writes=[y_g])
                for tt in range(NT):
                    s4, yb = ss[tt % 2], ynb[tt % 2]
                    y_g = y_gt[tt]
                    P.op("act", _mk(S.activation, out=sq[:], in_=y_g[:, tt, :], func=AF.Square), reads=[y_g], writes=[sq])
                    P.op("dve", _mk(V.tensor_reduce, out=s4[:, 0:1], in_=sq[:], axis=AX.X, op=ALU.add), reads=[sq], writes=[s4])
                    P.op("dve", _mk(V.tensor_scalar, out=s4[:, 1:2], in0=s4[:, 0:1], scalar1=1.0 / 1024.0, scalar2=RMS_EPS, op0=ALU.mult, op1=ALU.add), reads=[s4], writes=[s4])
                    P.op("act", _mk(S.sqrt, out=s4[:, 2:3], in_=s4[:, 1:2]), reads=[s4], writes=[s4])
                    P.op("dve", _mk(V.reciprocal, out=s4[:, 3:4], in_=s4[:, 2:3]), reads=[s4], writes=[s4])
                    P.op("dve", _mk(V.scalar_tensor_tensor, out=yb[:], in0=y_g[:, tt, :], scalar=s4[:, 3:4], in1=nwg[:], op0=ALU.mult, op1=ALU.mult),
                         reads=[y_g, s4, nwg], writes=[yb])
                    dma_store(cx, "sp", YN, YN[tt * 128:(tt + 1) * 128, g * 1024:(g + 1) * 1024], yb, yb[:])
            P.barrier()
            P.emit()
        with contextlib.ExitStack() as stk2:
            KI = DI // 128
            ynT = P.sb([128, KI, 512], BF16, "ynT", stk2)
            ynt = [P.sb([128, DI], BF16, "ynt", stk2) for _ in range(2)]
            wo = [P.sb([128, KI, 256], BF16, "wo", stk2) for _ in range(2)]
            xin = [P.sb([128, 256], F32, "xinb", stk2) for _ in range(4)]
            yo = [P.sb([128, 256], F32, "yob", stk2) for _ in range(4)]
            nwo = 0
            for half in range(2):
                for t4 in range(4):
                    tt = half * 4 + t4
                    yt_ = ynt[t4 % 2]
                    dma_load(cx, "sp", yt_, yt_[:], YN, YN[tt * 128:(tt + 1) * 128, :])
                    for q in range(8):
                        bank = cx.banks[q]
                        bv = bank[:].bitcast(BF16).rearrange("p (a b) -> p a b", a=8)
                        fns = [_mk(nc.tensor.transpose, bv[:, j, :], yt_[:, (q * 8 + j) * 128:(q * 8 + j + 1) * 128], cx.identb[:]) for j in range(8)]
                        P.op("pe", fns, reads=[yt_, cx.identb], writes=[bank])
                        if q % 2 == 0:
                            P.op("act", _mk(S.copy, out=ynT[:, q * 8:(q + 1) * 8, t4 * 128:(t4 + 1) * 128], in_=bv), reads=[bank], writes=[ynT])
                        else:
                            P.op("dve", _mk(V.tensor_copy, out=ynT[:, q * 8:(q + 1) * 8, t4 * 128:(t4 + 1) * 128], in_=bv), reads=[bank], writes=[ynT])
                for cc in range(D // 256):
                    w = wo[nwo % 2]
                    nwo += 1
                    load_w_cols(cx, w, w_out, cc * 256, 256, KI)
                    for t4 in range(4):
                        tt = half * 4 + t4
                        i = cc * 4 + t4
                        bank = cx.banks[i % 8]
                        xi, y = xin[i % 4], yo[i % 4]
                        dma_load(cx, "sp", xi, xi[:], x2h, x2h[HALO + tt * 128:HALO + (tt + 1) * 128, cc * 256:(cc + 1) * 256])
                        fns = [_mk(nc.tensor.matmul, bank[:, 0:256], ynT[:, k, t4 * 128:(t4 + 1) * 128], w[:, k, :], start=(k == 0), stop=(k == KI - 1)) for k in range(KI)]
                        P.op("pe", fns, reads=[ynT, w], writes=[bank])
                        P.op("dve", _mk(V.scalar_tensor_tensor, out=y[:], in0=xi[:], scalar=ALPHA, in1=bank[:, 0:256], op0=ALU.mult, op1=ALU.add),
                             reads=[xi, bank], writes=[y])
                        dma_store(cx, "sp", Y, Y[tt * 128:(tt + 1) * 128, cc * 256:(cc + 1) * 256], y, y[:])
            P.barrier()
            P.emit()


def _moe_io(P, sfx=""):
    return dict(wr=_din(P, "w_router", [D, NE]), rb=_din(P, "rb_bc", [128, NE]), offs=_din(P, "offs", [128, NE]), tokid=_din(P, "tokid", [128, NT, 2]),
                wie=_din(P, "w_in_e", [NE, D, 2 * DFF]), woe=_din(P, "w_out_e", [NE, DFF, D]),
                g1=_din(P, "g1_bc", [128, D]), b1=_din(P, "b1_bc", [128, D]), g2=_din(P, "g2_bc", [128, D]), b2=_din(P, "b2_bc", [128, D]))


def _post_mixer(cx, Y, io, XOUT):
    P = cx.P
    X1 = P.dram("X1s", [T, D], F32)
    X1b = P.dram("X1bs", [T, D], BF16)
    YB = P.dram("YBs", [NE * CAP, D], F32)
    Y2 = P.dram("Y2s", [T, D], F32)
    stage_ln(cx, Y, io["g1"], io["b1"], X1, X1b)
    stage_moe(cx, X1, X1b, io["wr"], io["rb"], io["offs"], io["tokid"], io["wie"], io["woe"], YB, Y2)
    stage_ln(cx, Y2, io["g2"], io["b2"], XOUT)


def _din(P, name, shape, dt=F32):
    return P.dram(name, shape, dt, kind="ExternalInput")


def _dout(P, name, shape, dt=F32):
    return P.dram(name, shape, dt, kind="ExternalOutput")


FUSED = False


def all_gather(cx, src, dst):
    nc = cx.nc
    cx.P.dma("pool", _mk(nc.gpsimd.collective_compute, "AllGather", ALU.bypass, replica_groups=[list(range(NCORES))],
                         ins=[src[:, :]], outs=[dst[:, :]]), dst, reads=[src], writes=[dst], inc=1)


_WSPEC = {
    "p_w_in": ([D // 128, 128, KC, 128], 0, 128),
    "p_w_group": ([4, 8, 128, 8, 128], 0, 128),
    "p_w_out": ([D // 512, 128, KC, 512], 0, 512),
    "w_in_e0": ([NE, 6, 128, KC, 256], 0, 256), "w_in_e1": ([NE, 6, 128, KC, 256], 0, 256),
    "w_out_e0": ([NE, 4, 128, 6, 1024], 0, 1024), "w_out_e1": ([NE, 4, 128, 6, 1024], 0, 1024),
    "s_w_z": ([DI // 256, 128, KC, 256], 0, 256),
    "s_w_x": ([81, 128, KC, 128], DI, 128),
    "s_w_out": ([D // 256, 128, DI // 128, 256], 0, 256),
}


def _tw(P, name):
    shape, col0, cw = _WSPEC[name]
    return TW(_din(P, name, shape), col0, cw)


def _host_w(inp, names):
    f = lambda a: np.asarray(a, np.float32)
    out = {}
    for n in names:
        if n == "p_w_in":
            out[n] = tile_w(f(inp["pool_w_in"][0]), 128)
        elif n == "p_w_group":
            out[n] = tile_w(f(inp["pool_w_group"][0]), 128)
        elif n == "p_w_out":
            out[n] = tile_w(f(inp["pool_w_out"][0]), 512)
        elif n.startswith("w_in_e"):
            out[n] = tile_w(f(inp["moe_w_in"][int(n[-1])]), 256)
        elif n.startswith("w_out_e"):
            out[n] = tile_w(f(inp["moe_w_out"][int(n[-1])]), 1024)
        elif n == "s_w_z":
            out[n] = tile_w(f(inp["ssd_w_in"][0])[:, :DI], 256)
        elif n == "s_w_x":
            out[n] = tile_w(f(inp["ssd_w_in"][0])[:, DI:], 128)
        elif n == "s_w_out":
            out[n] = tile_w(f(inp["ssd_w_out"][0]), 256)
    return out


def _small_inputs(P):
    return dict(wr=_din(P, "w_router", [D, NE]), rb=_din(P, "rb_bc", [128, NE]), offs=_din(P, "offs", [128, NE]), tokid=_din(P, "tokid", [128, NT, 2]),
                lnp=_din(P, "ln_bc", [8, 128, D]))


def _ssd_small(P):
    return dict(cw=_din(P, "convw_fm", [128, 80, 4]), cb=_din(P, "convb_fm", [128, 80]), dtb=_din(P, "dtb_bc", [128, NH]), al=_din(P, "alog_bc", [128, NH]),
                ds=_din(P, "dsk_bc", [128, NH]))


def _layer0(cx, xh, sm, W, Y, X1, X1b, YB, X2dst):
    P = cx.P
    p_sc = _din(P, "p_scale_fm", [128, KC])
    p_rf = _din(P, "p_rfix", [128, 4, 16])
    stage_pool(cx, xh, W["p_w_in"], W["p_w_group"], p_sc, p_rf, W["p_w_out"], Y)
    stage_ln(cx, Y, _LnView(sm["lnp"], 0), _LnView(sm["lnp"], 1), X1, X1b)
    stage_moe(cx, X1, X1b, sm["wr"], sm["rb"], sm["offs"], sm["tokid"], W["w_in_e0"], W["w_out_e0"], YB, Y)
    stage_ln(cx, Y, _LnView(sm["lnp"], 2), _LnView(sm["lnp"], 3), X2dst)


def _layer1_tail(cx, sm, W, Y, X1, X1b, YB, OUT):
    stage_ln(cx, Y, _LnView(sm["lnp"], 4), _LnView(sm["lnp"], 5), X1, X1b)
    stage_moe(cx, X1, X1b, sm["wr"], sm["rb"], sm["offs"], sm["tokid"], W["w_in_e1"], W["w_out_e1"], YB, Y)
    stage_ln(cx, Y, _LnView(sm["lnp"], 6), _LnView(sm["lnp"], 7), OUT)


def build_fused():
    nc = bass.Bass("TRN2", target_bir_lowering=False)
    cx = Ctx(nc)
    P = cx.P
    xh = _din(P, "xh", [T + HALO, D])
    sm = _small_inputs(P)
    ss = _ssd_small(P)
    W = {n: _tw(P, n) for n in _WSPEC}
    s_wi = WMulti([W["s_w_z"], W["s_w_x"]])
    nwb = _din(P, "normw_bc", [128, DI])
    Wk = _din(P, "Wk", [NCORES, NCORES, 128])
    negm = _din(P, "negm", [128, NCORES])
    hidx = _din(P, "halo_idx", [HALO, 1], I32)
    OUT = _dout(P, "OUT", [T, D])
    Y = P.dram("Ys", [T, D], F32)
    X1 = P.dram("X1s", [T, D], F32)
    X1b = P.dram("X1bs", [T, D], BF16)
    YB = P.dram("YBs", [NE * CAP, D], F32)
    X2H = P.dram("X2Hs", [T + HALO, D], F32)
    hb_in = P.dram("hb_in", [HALO, D], F32)
    hb_all = P.dram("hb_all", [NCORES * HALO + HALO, D], F32)
    hb_g = P.dram("hb_g", [NCORES * HALO, D], F32)
    YL = P.dram("YLs", [T, DI], F32)
    SL = P.dram("SLs", [128, DI], F32)
    TOTC = P.dram("TOTCs", [128, NH], F32)
    tot_in = P.dram("tot_in", [1, NH], F32)
    EAG = P.dram("EAGs", [128, NT, NH], F32)
    CTd = P.dram("CTds", [NG, 128, T], BF16)
    ACTd = P.dram("ACTds", [NH, T], F32)
    S_all = P.dram("S_alls", [NCORES * 128, DI], F32)
    TOT_all = P.dram("TOT_alls", [NCORES, NH], F32)
    YN = P.dram("YNs", [T, DI], BF16)
    _layer0(cx, xh, sm, W, Y, X1, X1b, YB, _RowView(X2H, HALO))
    with contextlib.ExitStack() as stk:
        zt = P.sb([HALO, D], F32, "hz", stk)
        ht = P.sb([HALO, D], F32, "ht", stk)
        hi = P.sb([HALO, 1], I32, "hi", stk)
        P.op("dve", _mk(nc.vector.memset, zt[:], 0.0), writes=[zt])
        dma_store(cx, "sp", hb_all, hb_all[NCORES * HALO:NCORES * HALO + HALO, :], zt, zt[:])
        dma_load(cx, "sp", ht, ht[:], X2H, X2H[T:T + HALO, :])
        dma_store(cx, "sp", hb_in, hb_in[:, :], ht, ht[:])
        all_gather(cx, hb_in, hb_g)
        for r0 in range(0, NCORES * HALO, HALO):
            dma_load(cx, "sp", ht, ht[:], hb_g, hb_g[r0:r0 + HALO, :])
            dma_store(cx, "sp", hb_all, hb_all[r0:r0 + HALO, :], ht, ht[:])
        dma_load(cx, "sp", hi, hi[:], hidx, hidx[:])
        P.barrier()
        P.dma("pool", _mk(nc.gpsimd.indirect_dma_start, out=ht[:], out_offset=None, in_=hb_all[:, :],
                          in_offset=bass.IndirectOffsetOnAxis(ap=hi[:, 0:1], axis=0)), ht, reads=[hb_all, hi], writes=[ht])
        dma_store(cx, "sp", X2H, X2H[0:HALO, :], ht, ht[:])
        P.barrier()
        P.emit()
    stage_ssd_a(cx, X2H, s_wi, ss["cw"], ss["cb"], ss["dtb"], ss["al"], ss["ds"], YL, SL, TOTC, EAG, CTd, ACTd)
    with contextlib.ExitStack() as stk:
        tt_ = P.sb([1, NH], F32, "tt_", stk)
        dma_load(cx, "sp", tt_, tt_[:], TOTC, TOTC[0:1, :])
        dma_store(cx, "sp", tot_in, tot_in[:, :], tt_, tt_[:])
        all_gather(cx, SL, S_all)
        all_gather(cx, tot_in, TOT_all)
        P.barrier()
        P.emit()
    stage_ssd_b(cx, X2H, s_wi, W["s_w_out"], YL, EAG, CTd, _S3View(S_all), TOT_all, Wk, negm, nwb, YN, Y)
    _layer1_tail(cx, sm, W, Y, X1, X1b, YB, OUT)
    P.wait_all("sp", [OUT])
    P.emit()
    return nc


def build_l0():
    nc = bass.Bass("TRN2", target_bir_lowering=False)
    cx = Ctx(nc)
    P = cx.P
    xh = _din(P, "xh", [T + HALO, D])
    sm = _small_inputs(P)
    W = {n: _tw(P, n) for n in ("p_w_in", "p_w_group", "p_w_out", "w_in_e0", "w_out_e0")}
    X2 = _dout(P, "X2", [T, D])
    Y = P.dram("Ys", [T, D], F32)
    X1 = P.dram("X1s", [T, D], F32)
    X1b = P.dram("X1bs", [T, D], BF16)
    YB = P.dram("YBs", [NE * CAP, D], F32)
    _layer0(cx, xh, sm, W, Y, X1, X1b, YB, X2)
    P.wait_all("sp", [X2])
    P.emit()
    return nc


def build_ssd_a():
    nc = bass.Bass("TRN2", target_bir_lowering=False)
    cx = Ctx(nc)
    P = cx.P
    x2h = _din(P, "x2h", [T + HALO, D])
    ss = _ssd_small(P)
    s_wi = WMulti([_tw(P, "s_w_x")])
    YL = _dout(P, "YL", [T, DI])
    SL = _dout(P, "SL", [128, DI])
    TOTC = _dout(P, "TOTC", [128, NH])
    EAG = _dout(P, "EAG", [128, NT, NH])
    CTd = _dout(P, "CTd", [NG, 128, T], BF16)
    ACTd = P.dram("ACTd", [NH, T], F32)
    stage_ssd_a(cx, x2h, s_wi, ss["cw"], ss["cb"], ss["dtb"], ss["al"], ss["ds"], YL, SL, TOTC, EAG, CTd, ACTd)
    P.wait_all("sp", [YL, SL, TOTC, EAG, CTd])
    P.emit()
    return nc


def build_l1b():
    nc = bass.Bass("TRN2", target_bir_lowering=False)
    cx = Ctx(nc)
    P = cx.P
    x2h = _din(P, "x2h", [T + HALO, D])
    sm = _small_inputs(P)
    W = {n: _tw(P, n) for n in ("s_w_z", "s_w_out", "w_in_e1", "w_out_e1")}
    YL = _din(P, "YL", [T, DI])
    EAG = _din(P, "EAG", [128, NT, NH])
    CTd = _din(P, "CTd", [NG, 128, T], BF16)
    S_all = _din(P, "S_all", [NCORES, 128, DI])
    TOT_all = _din(P, "TOT_all", [NCORES, NH])
    Wk = _din(P, "Wk", [NCORES, NCORES, 128])
    negm = _din(P, "negm", [128, NCORES])
    nwb = _din(P, "normw_bc", [128, DI])
    OUT = _dout(P, "OUT", [T, D])
    YN = P.dram("YNs", [T, DI], BF16)
    Y = P.dram("Ys", [T, D], F32)
    X1 = P.dram("X1s", [T, D], F32)
    X1b = P.dram("X1bs", [T, D], BF16)
    YB = P.dram("YBs", [NE * CAP, D], F32)
    stage_ssd_b(cx, x2h, WMulti([W["s_w_z"]]), W["s_w_out"], YL, EAG, CTd, S_all, TOT_all, Wk, negm, nwb, YN, Y)
    _layer1_tail(cx, sm, W, Y, X1, X1b, YB, OUT)
    P.wait_all("sp", [OUT])
    P.emit()
    return nc


class _ViewBase:
    def __init__(self, base):
        object.__setattr__(self, "base", base)

    def __getattr__(self, k):
        return getattr(object.__getattribute__(self, "base"), k)

    def __setattr__(self, k, v):
        setattr(object.__getattribute__(self, "base"), k, v)


class _RowView(_ViewBase):
    def __init__(self, base, row0):
        super().__init__(base)
        object.__setattr__(self, "row0", row0)

    def __getitem__(self, key):
        r0 = object.__getattribute__(self, "row0")
        base = object.__getattribute__(self, "base")
        if not isinstance(key, tuple):
            key = (key, slice(None))
        rs = key[0]
        start = (rs.start or 0) + r0
        stop = (rs.stop if rs.stop is not None else T) + r0
        return base[(slice(start, stop),) + tuple(key[1:])]


class _LnView(_ViewBase):
    def __init__(self, base, idx):
        super().__init__(base)
        object.__setattr__(self, "idx", idx)

    def __getitem__(self, key):
        base = object.__getattribute__(self, "base")
        return base[object.__getattribute__(self, "idx")][key]


class _S3View(_ViewBase):
    def __getitem__(self, j):
        base = object.__getattribute__(self, "base")
        return base[j * 128:(j + 1) * 128, :]


def _bc(v):
    v = np.asarray(v, np.float32)
    return np.ascontiguousarray(np.broadcast_to(v, (128,) + v.shape))


def _halo(xfull, c):
    xh = np.zeros((T + HALO, D), np.float32)
    lo = c * T - HALO
    if lo >= 0:
        xh[:] = xfull[lo:lo + T + HALO]
    else:
        xh[HALO:] = xfull[0:T]
    return xh


def _core_consts(k):
    rf = np.zeros((128, 4, 16), np.float32)
    pos = k * T + np.arange(16) + 1
    for gi, w in enumerate(POOL_W):
        rf[:, gi, :] = (1.0 / np.minimum(pos, w))[None, :]
    Wk = np.zeros((NCORES, NCORES, 128), np.float32)
    negm = np.zeros((128, NCORES), np.float32)
    for j in range(NCORES):
        for i in range(NCORES):
            if j < i < k:
                Wk[i, j, :] = 1.0
        if not j < k:
            negm[:, j] = -1e30
    hidx = (np.arange(HALO) + ((k - 1) * HALO if k > 0 else NCORES * HALO)).astype(np.int32).reshape(HALO, 1)
    return rf, Wk, negm, hidx


def kernel(**inp):
    cores = list(range(NCORES))
    f = lambda a: np.ascontiguousarray(a, np.float32)
    x = f(inp["x"])[0]
    tok = np.zeros((128, NT, 2), np.float32)
    tok[:, :, 0] = np.arange(128)[:, None]
    tok[:, :, 1] = np.arange(NT)[None, :]
    conv_w = f(inp["ssd_conv_w"][0])
    conv_b = f(inp["ssd_conv_b"][0])
    lnb = np.stack([_bc(inp[k][l]) for l in range(2) for k in ("ln_mix_g", "ln_mix_b", "ln_ffn_g", "ln_ffn_b")], 0)
    small = {"w_router": f(inp["moe_w_router"]), "rb_bc": _bc(inp["moe_router_bias"]),
             "offs": _bc((np.arange(NE) * CAP + 1).astype(np.float32)), "tokid": tok, "ln_bc": lnb}
    ssd_small = {"convw_fm": np.ascontiguousarray(conv_w.T.reshape(80, 128, 4).transpose(1, 0, 2)),
                 "convb_fm": np.ascontiguousarray(conv_b.reshape(80, 128).T), "dtb_bc": _bc(inp["ssd_dt_bias"][0]), "alog_bc": _bc(inp["ssd_a_log"][0]),
                 "dsk_bc": _bc(inp["ssd_d"][0])}
    pscale = np.ascontiguousarray(f(inp["pool_scale"][0]).reshape(KC, 128).T)
    consts = [_core_consts(k) for k in cores]
    if FUSED:
        shared = dict(small)
        shared.update(ssd_small)
        shared.update(_host_w(inp, list(_WSPEC)))
        shared.update({"p_scale_fm": pscale, "normw_bc": _bc(inp["ssd_norm_w"][0])})
        maps = []
        for k in cores:
            rf, Wk, negm, hidx = consts[k]
            m = dict(shared)
            m.update({"xh": _halo(x, k), "p_rfix": rf, "Wk": Wk, "negm": negm, "halo_idx": hidx})
            maps.append(m)
        r = run_bass_kernel_spmd(build_fused(), maps, core_ids=cores)
        out = np.concatenate([np.asarray(r.results[c]["OUT"]) for c in cores], 0)
        return out[None].astype(np.float32)
    shared = dict(small)
    shared.update(_host_w(inp, ["p_w_in", "p_w_group", "p_w_out", "w_in_e0", "w_out_e0"]))
    shared["p_scale_fm"] = pscale
    maps = []
    for k in cores:
        m = dict(shared)
        m.update({"xh": _halo(x, k), "p_rfix": consts[k][0]})
        maps.append(m)
    r = run_bass_kernel_spmd(build_l0(), maps, core_ids=cores)
    x2 = np.concatenate([np.asarray(r.results[c]["X2"]) for c in cores], 0)
    del r, maps, shared
    shared = dict(ssd_small)
    shared.update(_host_w(inp, ["s_w_x"]))
    x2h = [_halo(x2, c) for c in cores]
    maps = []
    for k in cores:
        m = dict(shared)
        m["x2h"] = x2h[k]
        maps.append(m)
    ra = run_bass_kernel_spmd(build_ssd_a(), maps, core_ids=cores)
    S_all = np.stack([np.asarray(ra.results[c]["SL"]) for c in cores], 0)
    TOT_all = np.stack([np.asarray(ra.results[c]["TOTC"])[0] for c in cores], 0)
    del maps, shared
    shared = dict(small)
    shared.update(_host_w(inp, ["s_w_z", "s_w_out", "w_in_e1", "w_out_e1"]))
    shared.update({"S_all": S_all, "TOT_all": TOT_all, "normw_bc": _bc(inp["ssd_norm_w"][0])})
    maps = []
    for k in cores:
        m = dict(shared)
        o = ra.results[k]
        m.update({"x2h": x2h[k], "YL": np.asarray(o["YL"]), "EAG": np.asarray(o["EAG"]), "CTd": np.asarray(o["CTd"]), "Wk": consts[k][1], "negm": consts[k][2]})
        maps.append(m)
    rb = run_bass_kernel_spmd(build_l1b(), maps, core_ids=cores)
    out = np.concatenate([np.asarray(rb.results[c]["OUT"]) for c in cores], 0)
    return out[None].astype(np.float32)
```
